# Optimizing a Trainium2 kernel written in Bass

```python
import math
import jax
import jax.numpy as jnp
from jax import lax
import numpy as np

D_MODEL = 1024
BATCH = 8
SEQ = 2048
DEPTH = 2

CTX_LEN = 256
GRID_W = 64
RMS_EPS = 1e-6
N_MOD = 6

SHORT_CONV = 3
FILTER_BANDS = 8
FILTER_EMB = 1 + 2 * FILTER_BANDS
FILTER_HIDDEN = 64
FILTER_INNER = 2
DECAY_TARGET = 1e-2
FAST_DECAY_PCT = 0.3
SLOW_DECAY_PCT = 1.5

N_HEADS = 16
QK_NOPE = 64
QK_ROPE = 32
QK_HEAD = QK_NOPE + QK_ROPE
V_HEAD = 64
Q_LORA = 384
KV_LORA = 256
ROPE_AXIS = QK_ROPE // 2
ROPE_BASE = 10000.0
Q_BLOCK = 128

D_FF = 3584
N_EXPERTS = 8
TOP_K = 2

kernel_name = 'hybrid_hyena_mla_moe_flow_block'


def rmsnorm(x, g):
    xf = x.astype(jnp.float32)
    y = xf * lax.rsqrt(jnp.mean(xf * xf, axis=-1, keepdims=True) + RMS_EPS)
    return (y * g.astype(jnp.float32)).astype(x.dtype)


def modulate(h, shift, scale):
    return h * (1 + scale) + shift


def swiglu(h, w1, w3, w2):
    return (jax.nn.silu(h @ w1) * (h @ w3)) @ w2


def short_conv(z, w, b):
    L = z.shape[1]
    pad = SHORT_CONV // 2
    zp = jnp.pad(z, ((0, 0), (pad, SHORT_CONV - 1 - pad), (0, 0)))
    return sum(zp[:, j:j + L] * w[j] for j in range(SHORT_CONV)) + b


def hyena_filter(L, w0, b0, wi, bi, freq, wout):
    pos = jnp.arange(L, dtype=jnp.float32)
    t = (pos / max(L - 1, 1))[:, None]
    w = 2.0 * math.pi * pos / L
    f = jnp.linspace(1e-4, FILTER_BANDS - 1, FILTER_BANDS, dtype=jnp.float32)
    ang = w[:, None] * f[None, :]
    z = jnp.concatenate([t, jnp.cos(ang), -jnp.sin(ang)], axis=-1)
    fr = freq.astype(jnp.float32)
    h = jnp.sin(fr * (z @ w0.astype(jnp.float32) + b0.astype(jnp.float32)))
    for n in range(FILTER_INNER):
        h = jnp.sin(fr * (h @ wi[n].astype(jnp.float32) + bi[n].astype(jnp.float32)))
    h = h @ wout.astype(jnp.float32)
    D = wout.shape[-1] // 2
    deltas = jnp.abs(jnp.linspace(math.log(DECAY_TARGET) / SLOW_DECAY_PCT,
                                  math.log(DECAY_TARGET) / FAST_DECAY_PCT, D, dtype=jnp.float32))
    decay = jnp.exp(-t * deltas[None, :])
    h = h.reshape(L, 2, D) * decay[:, None, :]
    k_full = jnp.concatenate([h[:, 0], jnp.zeros((1, D), jnp.float32), h[1:, 1][::-1]], axis=0)
    return k_full / jnp.sum(jnp.abs(k_full), axis=0, keepdims=True)


def long_conv(v, k_full):
    L = v.shape[1]
    vf = jnp.fft.rfft(v.astype(jnp.float32), n=2 * L, axis=1)
    kf = jnp.fft.rfft(k_full, n=2 * L, axis=0)
    y = jnp.fft.irfft(vf * kf[None], n=2 * L, axis=1)[:, :L]
    return y.astype(v.dtype)


def hyena_mixer(h, p):
    in_w, in_b, sc_w, sc_b, f_w0, f_b0, f_wi, f_bi, f_freq, f_wout, f_bias, out_w, out_b = p
    L = h.shape[1]
    z = short_conv(h @ in_w + in_b, sc_w, sc_b)
    x0, x1, v = jnp.split(z, 3, axis=-1)
    v = v * x1
    k_full = hyena_filter(L, f_w0, f_b0, f_wi, f_bi, f_freq, f_wout)
    v = long_conv(v, k_full) + v * f_bias
    return (x0 * v) @ out_w + out_b


def axial_rope(L):
    rows = L // GRID_W
    row = jnp.broadcast_to(jnp.arange(rows, dtype=jnp.float32)[:, None], (rows, GRID_W)).reshape(L)
    col = jnp.broadcast_to(jnp.arange(GRID_W, dtype=jnp.float32)[None, :], (rows, GRID_W)).reshape(L)
    inv = ROPE_BASE ** (-jnp.arange(0, ROPE_AXIS, 2, dtype=jnp.float32) / ROPE_AXIS)
    ang = jnp.concatenate([row[:, None] * inv, col[:, None] * inv], axis=-1)
    return jnp.cos(ang), jnp.sin(ang)


def apply_rope(x, cos, sin):
    xf = x.astype(jnp.float32).reshape(x.shape[:-1] + (QK_ROPE // 2, 2))
    a, b = xf[..., 0], xf[..., 1]
    out = jnp.stack([a * cos - b * sin, a * sin + b * cos], axis=-1)
    return out.reshape(x.shape).astype(x.dtype)


def mla_q(h, wq_a, q_norm, wq_b, rope):
    B, L, _ = h.shape
    q = (rmsnorm(h @ wq_a, q_norm) @ wq_b).reshape(B, L, N_HEADS, QK_HEAD)
    q_nope, q_pe = q[..., :QK_NOPE], q[..., QK_NOPE:]
    if rope is not None:
        cos, sin = rope
        q_pe = apply_rope(q_pe, cos[None, :, None], sin[None, :, None])
    return jnp.concatenate([q_nope, q_pe], axis=-1)


def mla_kv(h, wkv_a, kv_norm, wkv_b, rope):
    B, L, _ = h.shape
    kv = h @ wkv_a
    c_kv = rmsnorm(kv[..., :KV_LORA], kv_norm)
    k_pe = kv[..., KV_LORA:]
    if rope is not None:
        cos, sin = rope
        k_pe = apply_rope(k_pe, cos[None], sin[None])
    kvb = (c_kv @ wkv_b).reshape(B, L, N_HEADS, QK_NOPE + V_HEAD)
    k = jnp.concatenate([kvb[..., :QK_NOPE],
                         jnp.broadcast_to(k_pe[:, :, None, :], (B, L, N_HEADS, QK_ROPE))], axis=-1)
    return k, kvb[..., QK_NOPE:]


def attention(q, k, v):
    B, Lq, H, Dq = q.shape
    nb = Lq // Q_BLOCK
    scale = 1.0 / math.sqrt(Dq)
    qb = q.reshape(B, nb, Q_BLOCK, H, Dq).transpose(1, 0, 2, 3, 4)

    def block(qblk):
        s = jnp.einsum('bqhd,bkhd->bhqk', qblk, k).astype(jnp.float32) * scale
        p = jax.nn.softmax(s, axis=-1).astype(v.dtype)
        return jnp.einsum('bhqk,bkhd->bqhd', p, v)

    o = lax.map(block, qb)
    return o.transpose(1, 0, 2, 3, 4).reshape(B, Lq, H, v.shape[-1])


def mla_mixer(hx, hc, p, rope, ctx_queries):
    wq_a, q_norm, wq_b, wkv_a, kv_norm, wkv_b, wo = p
    B, L, _ = hx.shape
    kc, vc = mla_kv(hc, wkv_a, kv_norm, wkv_b, None)
    kx, vx = mla_kv(hx, wkv_a, kv_norm, wkv_b, rope)
    qx = mla_q(hx, wq_a, q_norm, wq_b, rope)
    ox = attention(qx, jnp.concatenate([kc, kx], axis=1), jnp.concatenate([vc, vx], axis=1))
    yx = ox.reshape(B, L, N_HEADS * V_HEAD) @ wo
    yc = None
    if ctx_queries:
        qc = mla_q(hc, wq_a, q_norm, wq_b, None)
        oc = attention(qc, kc, vc)
        yc = oc.reshape(B, hc.shape[1], N_HEADS * V_HEAD) @ wo
    return yx, yc


def moe(h, router, w1, w3, w2):
    B, L, D = h.shape
    t = h.reshape(B * L, D)
    logits = (t @ router).astype(jnp.float32)
    vals, idx = lax.top_k(logits, TOP_K)
    wts = jax.nn.softmax(vals, axis=-1)
    gates = jnp.sum(jax.nn.one_hot(idx, N_EXPERTS, dtype=jnp.float32) * wts[..., None], axis=1)
    gates = gates.astype(t.dtype)
    out = jnp.zeros_like(t)
    for e in range(N_EXPERTS):
        out = out + gates[:, e:e + 1] * swiglu(t, w1[e], w3[e], w2[e])
    return out.reshape(B, L, D)


def setup_inputs(seed: int = 0) -> dict:
    key = jax.random.key(seed)
    ks = iter(jax.random.split(key, 48))
    nh = (DEPTH + 1) // 2
    nm = DEPTH // 2
    D = D_MODEL

    def nrm(shape, scale):
        return jax.random.normal(next(ks), shape, jnp.float32) * scale

    def gain(shape):
        return 1.0 + nrm(shape, 0.05)

    return {
        'x': nrm((BATCH, SEQ, D), 1.0),
        'c': nrm((BATCH, D), 1.0),
        'ctx': nrm((BATCH, CTX_LEN, D), 1.0),
        'c_ctx': nrm((D,), 1.0),
        'ada_w': nrm((DEPTH, D, N_MOD * D), 0.5 * D ** -0.5),
        'ada_b': nrm((DEPTH, N_MOD * D), 0.02),
        'norm_mix': gain((DEPTH, D)),
        'norm_ffn': gain((DEPTH, D)),
        'hy_in_w': nrm((nh, D, 3 * D), D ** -0.5),
        'hy_in_b': nrm((nh, 3 * D), 0.02),
        'hy_sc_w': nrm((nh, SHORT_CONV, 3 * D), SHORT_CONV ** -0.5),
        'hy_sc_b': nrm((nh, 3 * D), 0.02),
        'hy_f_w0': nrm((nh, FILTER_EMB, FILTER_HIDDEN), FILTER_EMB ** -0.5),
        'hy_f_b0': nrm((nh, FILTER_HIDDEN), 0.02),
        'hy_f_wi': nrm((nh, FILTER_INNER, FILTER_HIDDEN, FILTER_HIDDEN), FILTER_HIDDEN ** -0.5),
        'hy_f_bi': nrm((nh, FILTER_INNER, FILTER_HIDDEN), 0.02),
        'hy_f_freq': gain((nh, FILTER_HIDDEN)),
        'hy_f_wout': nrm((nh, FILTER_HIDDEN, 2 * D), FILTER_HIDDEN ** -0.5),
        'hy_f_bias': nrm((nh, D), 0.1),
        'hy_out_w': nrm((nh, D, D), D ** -0.5),
        'hy_out_b': nrm((nh, D), 0.02),
        'mla_wq_a': nrm((nm, D, Q_LORA), D ** -0.5),
        'mla_q_norm': gain((nm, Q_LORA)),
        'mla_wq_b': nrm((nm, Q_LORA, N_HEADS * QK_HEAD), Q_LORA ** -0.5),
        'mla_wkv_a': nrm((nm, D, KV_LORA + QK_ROPE), D ** -0.5),
        'mla_kv_norm': gain((nm, KV_LORA)),
        'mla_wkv_b': nrm((nm, KV_LORA, N_HEADS * (QK_NOPE + V_HEAD)), KV_LORA ** -0.5),
        'mla_wo': nrm((nm, N_HEADS * V_HEAD, D), (N_HEADS * V_HEAD) ** -0.5),
        'ffn_w1': nrm((nh, D, D_FF), D ** -0.5),
        'ffn_w3': nrm((nh, D, D_FF), D ** -0.5),
        'ffn_w2': nrm((nh, D_FF, D), D_FF ** -0.5),
        'moe_router': nrm((nm, D, N_EXPERTS), D ** -0.5),
        'moe_w1': nrm((nm, N_EXPERTS, D, D_FF), D ** -0.5),
        'moe_w3': nrm((nm, N_EXPERTS, D, D_FF), D ** -0.5),
        'moe_w2': nrm((nm, N_EXPERTS, D_FF, D), D_FF ** -0.5),
        'norm_final': gain((D,)),
    }


def reference(x, c, ctx, c_ctx, ada_w, ada_b, norm_mix, norm_ffn,
              hy_in_w, hy_in_b, hy_sc_w, hy_sc_b, hy_f_w0, hy_f_b0, hy_f_wi, hy_f_bi,
              hy_f_freq, hy_f_wout, hy_f_bias, hy_out_w, hy_out_b,
              mla_wq_a, mla_q_norm, mla_wq_b, mla_wkv_a, mla_kv_norm, mla_wkv_b, mla_wo,
              ffn_w1, ffn_w3, ffn_w2, moe_router, moe_w1, moe_w3, moe_w2, norm_final):
    rope = axial_rope(x.shape[1])
    for i in range(DEPTH):
        j = i // 2
        last = i == DEPTH - 1
        mod_x = (jax.nn.silu(c) @ ada_w[i] + ada_b[i])[:, None, :]
        mod_c = (jax.nn.silu(c_ctx) @ ada_w[i] + ada_b[i])[None, None, :]
        sh1x, sc1x, g1x, sh2x, sc2x, g2x = jnp.split(mod_x, N_MOD, axis=-1)
        sh1c, sc1c, g1c, sh2c, sc2c, g2c = jnp.split(mod_c, N_MOD, axis=-1)

        hx = modulate(rmsnorm(x, norm_mix[i]), sh1x, sc1x)
        hc = modulate(rmsnorm(ctx, norm_mix[i]), sh1c, sc1c)
        if i % 2 == 0:
            hp = (hy_in_w[j], hy_in_b[j], hy_sc_w[j], hy_sc_b[j], hy_f_w0[j], hy_f_b0[j],
                  hy_f_wi[j], hy_f_bi[j], hy_f_freq[j], hy_f_wout[j], hy_f_bias[j],
                  hy_out_w[j], hy_out_b[j])
            yx = hyena_mixer(hx, hp)
            yc = None if last else hyena_mixer(hc, hp)
        else:
            mp = (mla_wq_a[j], mla_q_norm[j], mla_wq_b[j], mla_wkv_a[j], mla_kv_norm[j],
                  mla_wkv_b[j], mla_wo[j])
            yx, yc = mla_mixer(hx, hc, mp, rope, not last)
        x = x + g1x * yx
        if not last:
            ctx = ctx + g1c * yc

        hx = modulate(rmsnorm(x, norm_ffn[i]), sh2x, sc2x)
        if i % 2 == 0:
            x = x + g2x * swiglu(hx, ffn_w1[j], ffn_w3[j], ffn_w2[j])
        else:
            x = x + g2x * moe(hx, moe_router[j], moe_w1[j], moe_w3[j], moe_w2[j])
        if not last:
            hc = modulate(rmsnorm(ctx, norm_ffn[i]), sh2c, sc2c)
            if i % 2 == 0:
                ctx = ctx + g2c * swiglu(hc, ffn_w1[j], ffn_w3[j], ffn_w2[j])
            else:
                ctx = ctx + g2c * moe(hc, moe_router[j], moe_w1[j], moe_w3[j], moe_w2[j])
    return rmsnorm(x, norm_final)
```

```python
import contextlib
import numpy as np
import ml_dtypes
import concourse.bass as bass
import concourse.mybir as mybir
from concourse.bass_utils import run_bass_kernel_spmd

F32 = mybir.dt.float32
BF16 = mybir.dt.bfloat16
I32 = mybir.dt.int32
AF = mybir.ActivationFunctionType
ALU = mybir.AluOpType

D = 1024
L = 2048
LC = 256
NTOK = L + LC
DFF = 3584
NE = 8
TBS = [(0, 512), (512, 512), (1024, 512), (1536, 512), (2048, 256)]
TWO_PI = float(2 * np.pi)


class Buf:
    __slots__ = ("w", "r")

    def __init__(self):
        self.w = None
        self.r = {}


class Sched:
    ENG = ("pe", "act", "dve", "pool", "sp")
    NDQ = 8

    def __init__(self, nc, stack):
        self.nc = nc
        self.prog = {e: [] for e in self.ENG}
        self.cnt = {e: 0 for e in self.ENG}
        self.sem = {e: stack.enter_context(nc.semaphore(f"s_{e}")) for e in ("pe", "act", "dve", "pool")}
        self.dq = {}
        for q in ("sp", "pool"):
            self.dq[q] = dict(n=0, sems=[stack.enter_context(nc.semaphore(f"d_{q}{i}")) for i in range(self.NDQ)])
        self.seen = {e: {} for e in self.ENG}

    def _semobj(self, key):
        return self.sem[key[1]] if key[0] == "e" else self.dq[key[1]]["sems"][key[2]]

    def _collect(self, eng, reads, writes):
        waits = {}

        def need(key, val):
            if key == ("e", "pe") and eng == "pe":
                return
            if self.seen[eng].get(key, 0) >= val:
                return
            if waits.get(key, 0) < val:
                waits[key] = val

        for b in reads:
            if b.w is not None:
                need(*b.w)
        for b in writes:
            if b.w is not None:
                need(*b.w)
            for k, v in b.r.items():
                if k == ("e", eng):
                    continue
                need(k, v)
        for key, val in waits.items():
            self.seen[eng][key] = val
        return [(self._semobj(k), v) for k, v in waits.items()]

    def op(self, eng, fn, reads=(), writes=()):
        waits = self._collect(eng, reads, writes)
        self.cnt[eng] += 1
        key, val = ("e", eng), self.cnt[eng]
        for b in reads:
            if b.r.get(key, 0) < val:
                b.r[key] = val
        for b in writes:
            b.w = (key, val)
            b.r = {}
        self.prog[eng].append((waits, fn, self.sem[eng], 1))

    def dma(self, q, out, in_, reads=(), writes=()):
        d = self.dq[q]
        n = d["n"]
        d["n"] += 1
        slot = n % self.NDQ
        val = 16 * (n // self.NDQ + 1)
        waits = self._collect(q, reads, writes)
        key = ("d", q, slot)
        if n >= self.NDQ and self.seen[q].get(key, 0) < val - 16:
            waits.append((d["sems"][slot], val - 16))
            self.seen[q][key] = val - 16
        for b in reads:
            b.r[key] = val
        for b in writes:
            b.w = (key, val)
            b.r = {}
        self.prog[q].append((waits, (lambda e: e.dma_start(out=out, in_=in_)), d["sems"][slot], 16))

    def flush_dma(self):
        for q, d in self.dq.items():
            n = d["n"]
            waits = []
            for slot in range(self.NDQ):
                cs = (n - slot + self.NDQ - 1) // self.NDQ if n > slot else 0
                if cs > 0:
                    key = ("d", q, slot)
                    val = 16 * cs
                    if self.seen[q].get(key, 0) < val:
                        waits.append((d["sems"][slot], val))
                        self.seen[q][key] = val
            if waits:
                self.prog[q].append((waits, None, None, 0))

    def emit(self):
        self.flush_dma()
        prog = self.prog
        self.prog = {e: [] for e in self.ENG}
        with self.nc.Block() as block:
            def mk(e):
                def body(eng):
                    for waits, fn, sem, amt in prog[e]:
                        for s, v in waits:
                            eng.wait_ge(s, v)
                        if fn is not None:
                            fn(eng).then_inc(sem, amt)
                return body
            block.tensor(mk("pe"))
            block.scalar(mk("act"))
            block.vector(mk("dve"))
            block.gpsimd(mk("pool"))
            block.sync(mk("sp"))


def MMS(mms):
    def fn(e):
        ins = None
        for (o, l, r, st, sp) in mms:
            ins = e.matmul(o, l, r, start=st, stop=sp)
        return ins
    return fn


def TR(out, in_, ident):
    return lambda e: e.transpose(out, in_, ident)


def ACT(out, in_, func, scale=None, bias=None):
    kw = {}
    if scale is not None:
        kw["scale"] = scale
    if bias is not None:
        kw["bias"] = bias
    return lambda e: e.activation(out=out, in_=in_, func=func, **kw)


def TS(out, in0, s1, s2, op0, op1=None):
    if op1 is None:
        return lambda e: e.tensor_scalar(out=out, in0=in0, scalar1=s1, scalar2=None, op0=op0)
    return lambda e: e.tensor_scalar(out=out, in0=in0, scalar1=s1, scalar2=s2, op0=op0, op1=op1)


def TT(out, in0, in1, op):
    return lambda e: e.tensor_tensor(out=out, in0=in0, in1=in1, op=op)


def STT(out, in0, scalar, in1, op0, op1):
    return lambda e: e.scalar_tensor_tensor(out=out, in0=in0, scalar=scalar, in1=in1, op0=op0, op1=op1)


def CP(out, in_):
    return lambda e: e.tensor_copy(out=out, in_=in_)


def MSET(ap, v):
    return lambda e: e.memset(ap, v)


VEC_SPEC = [("c2", 16), ("adab0", 48), ("adab1", 48), ("nmix0", 8), ("nmix1", 8), ("nffn0", 8), ("nffn1", 8),
            ("inb", 24), ("scw0", 24), ("scw1", 24), ("scw2", 24), ("scb", 24), ("outb", 8), ("qnorm", 3),
            ("kvnorm", 2), ("nfin", 8), ("fb0", 1), ("fbi0", 1), ("fbi1", 1), ("ffreq", 1), ("eps", 1),
            ("tnx", 16), ("tnc", 2)]
VOFF = {}
_o = 0
for _n, _c in VEC_SPEC:
    VOFF[_n] = (_o, _c)
    _o += _c
NV = _o


def build(stop_after=99, dbg=False):
    nc = bass.Bass("TRN2", target_bir_lowering=False)

    def din(name, shape, dt=F32):
        return nc.dram_tensor(name, list(shape), dt, kind="ExternalInput").ap()

    def dscr(name, shape, dt):
        return nc.dram_tensor(name, list(shape), dt, kind="Internal").ap()

    xT_d = din("xT", [D, L])
    cT_d = din("cT", [D, LC])
    vec_d = din("vec", [128, NV])
    fbr_d = din("fbias_row", [1, D])
    dbc_d = din("delta_bc", [128, D])
    zpos_d = {L: din("zpos_x", [17, L]), LC: din("zpos_c", [17, LC])}
    dftf_d = {L: din("dftf_x", [16, 128, 2, 16, 128], BF16), LC: din("dftf_c", [2, 128, 2, 2, 128], BF16)}
    dfti_d = {L: din("dfti_x", [16, 128, 2, L], BF16), LC: din("dfti_c", [2, 128, 2, LC], BF16)}
    ropeC_d = din("ropeC", [32, L], BF16)
    ropeS_d = din("ropeS", [32, L], BF16)
    sel_d = din("sel", [8, 8, 128])
    ada_w_d = din("ada_w", [2, D, 6 * D])
    in_w_d = din("hy_in_w", [D, 3 * D])
    fw0_d = din("hy_f_w0", [17, 64])
    fwi_d = din("hy_f_wi", [2, 64, 64])
    fwout_d = din("hy_f_wout", [64, 2 * D])
    out_w_d = din("hy_out_w", [D, D])
    w1_d = din("ffn_w1", [D, DFF])
    w3_d = din("ffn_w3", [D, DFF])
    w2_d = din("ffn_w2", [DFF, D])
    wqa_d = din("wq_a", [D, 384])
    wqb2_d = din("wq_b2", [384, 16, 128])
    ropeCS_d = din("ropeCS", [128, L], BF16)
    shift_d = din("shiftm", [128, 96], BF16)
    wkva_d = din("wkv_a", [D, 256])
    wkpe_d = din("wkpe", [D, 96])
    wkpes_d = din("wkpe_sw", [D, 96])
    wkbn_d = din("wkb_nope", [256, 16, 64])
    wvb_d = din("wvb", [256, 1024])
    wo_d = din("wo", [16, 64, D])
    rout_d = din("router", [D, NE])
    mw1_d = din("moe_w1", [NE, D, DFF]) if stop_after >= 6 else None
    mw3_d = din("moe_w3", [NE, D, DFF]) if stop_after >= 6 else None
    mw2_d = din("moe_w2", [NE, DFF, D]) if stop_after >= 6 else None
    yT_d = nc.dram_tensor("yT", [D, L], F32, kind="ExternalOutput").ap()
    dbg_d = nc.dram_tensor("dbg", [D, NTOK], F32, kind="ExternalOutput").ap() if dbg else None

    pq_d = {L: dscr("pq_x", [16, 128, 2, D], BF16), LC: dscr("pq_c", [2, 128, 2, D], BF16)}
    x0_d = dscr("x0_s", [8, 128, NTOK], BF16)
    u_d = dscr("u_s", [8, 128, NTOK], BF16)
    q_d = dscr("q_s", [16, 96, L], BF16)
    k_d = dscr("k_s", [16, 96, NTOK], BF16)
    ao_d = dscr("ao_s", [16, 64, L], BF16)

    with contextlib.ExitStack() as gs:
        S = Sched(nc, gs)
        cnt = [0]

        def tile(st, name, shape, dt):
            cnt[0] += 1
            return st.enter_context(nc.sbuf_tensor(f"{name}_{cnt[0]}", list(shape), dt))

        PS = [gs.enter_context(nc.psum_tensor(f"ps{i}", [128, 512], F32)) for i in range(7)]
        PSB = gs.enter_context(nc.psum_tensor("psb", [128, 1024], BF16))
        PB = [Buf() for _ in range(7)]
        PBB = [Buf(), Buf()]
        PS6b = PS[6].bitcast(BF16)

        xs_d = dscr("x_spill", [8, 128, NTOK], F32)
        XH = {"t": None}
        xB = [[Buf() for _ in TBS] for _ in range(8)]
        vec = tile(gs, "vec", [128, NV], F32)
        vecB = Buf()
        mod = tile(gs, "mod", [128, 2, 48, 2], F32)
        modB = Buf()
        der = tile(gs, "der", [128, 2, 2, 3, 8], F32)
        derB = Buf()
        invn = tile(gs, "invn", [128, 2, 8], F32)
        invnB = Buf()
        ones_f = tile(gs, "ones_f", [128, 128], F32)
        ones_b = tile(gs, "ones_b", [128, 128], BF16)
        ident_f = tile(gs, "ident_f", [128, 128], F32)
        ident_b = tile(gs, "ident_b", [128, 128], BF16)
        cB = Buf()

        def V(name, j=0, n=1):
            o, c = VOFF[name]
            return vec[:, o + j:o + j + n]

        def V64(name):
            o, c = VOFF[name]
            return vec[0:64, o:o + 1]

        class WS:
            def __init__(self, st, name, shape, nst=2, nbf=2, cast=True, ceng="act"):
                self.cast = cast
                self.ceng = ceng
                self.st = [tile(st, name + "s", shape, F32) for _ in range(nst)]
                self.sB = [Buf() for _ in range(nst)]
                if cast:
                    self.bf = [tile(st, name + "b", shape, BF16) for _ in range(nbf)]
                    self.bB = [Buf() for _ in range(nbf)]
                self.i = 0

            def issue(self, src):
                i = self.i
                self.i += 1
                s, sb = self.st[i % len(self.st)], self.sB[i % len(self.st)]
                S.dma("sp", s[:], src, writes=[sb])
                return i

            def finish(self, i):
                s, sb = self.st[i % len(self.st)], self.sB[i % len(self.st)]
                b, bb = self.bf[i % len(self.bf)], self.bB[i % len(self.bf)]
                S.op("act", CPACT(b[:], s[:]), reads=[sb], writes=[bb])
                return b, bb

            def load(self, src, sl=None):
                i = self.i
                self.i += 1
                s, sb = self.st[i % len(self.st)], self.sB[i % len(self.st)]
                sv = s[:] if sl is None else sl(s)
                S.dma("sp", sv, src, writes=[sb])
                if not self.cast:
                    return s, sb
                b, bb = self.bf[i % len(self.bf)], self.bB[i % len(self.bf)]
                bv = b[:] if sl is None else sl(b)
                if self.ceng == "act":
                    S.op("act", CPACT(bv, sv), reads=[sb], writes=[bb])
                else:
                    S.op(self.ceng, CP(bv, sv), reads=[sb], writes=[bb])
                return b, bb

        def dump(k_list=range(8)):
            if dbg:
                for k in k_list:
                    S.dma("sp", dbg_d[k * 128:(k + 1) * 128, :], XH["t"][:, k, :], reads=xB[k])
                S.emit()

        with contextlib.ExitStack() as ph:
            S.dma("sp", vec[:], vec_d, writes=[vecB])
            S.op("pool", MSET(ones_f[:], 1.0), writes=[cB])
            S.op("pool", CP(ones_b[:], ones_f[:]), reads=[cB], writes=[cB])
            S.op("pool", MSET(ident_f[:], 1.0), reads=[cB], writes=[cB])
            S.op("pool", lambda e: e.affine_select(out=ident_f[:], in_=ident_f[:], pattern=[[-1, 128]],
                                                    compare_op=ALU.is_equal, fill=0.0, base=0, channel_multiplier=1),
                 reads=[cB], writes=[cB])
            S.op("pool", CP(ident_b[:], ident_f[:]), reads=[cB], writes=[cB])
            c2 = tile(gs, "c2", [128, 16], F32)
            c2B = Buf()
            S.op("act", ACT(c2[:], V("c2", 0, 16), AF.Silu), reads=[vecB], writes=[c2B])
            S.emit()

        PSBf = PSB.bitcast(F32)

        c2b = tile(gs, "c2b", [128, 16], BF16)
        S.op("act", CPACT(c2b[:], c2[:]), reads=[c2B], writes=[c2B])

        def ada_block(adaW, bi):
            l, cb = bi // 24, bi % 24
            wt, wb = adaW.load(ada_w_d[l][:, cb * 256:(cb + 1) * 256].rearrange("(k p) n -> p k n", p=128))
            mms = []
            for j in range(2):
                for k in range(8):
                    mms.append((PSBf[:, 2 * j:2 * j + 2], wt[:, k, j * 128:(j + 1) * 128], c2b[:, 2 * k:2 * k + 2], k == 0, k == 7))
            S.op("pe", MMS(mms), reads=[wb, c2B], writes=[PBB[0]])
            for j in range(2):
                jj = cb * 2 + j
                S.op("dve", TS(mod[:, l, jj, :], PSBf[:, 2 * j:2 * j + 2], V(f"adab{l}", jj), None, ALU.add),
                     reads=[PBB[0], vecB], writes=[modB])

        def ada_derived():
            for l in range(2):
                for s in range(2):
                    S.op("dve", STT(der[:, l, s, 0, :], mod[:, l, 8:16, s], 1.0, V(f"nmix{l}", 0, 8), ALU.add, ALU.mult),
                         reads=[modB, vecB], writes=[derB])
                    S.op("dve", STT(der[:, l, s, 1, :], mod[:, l, 32:40, s], 1.0, V(f"nffn{l}", 0, 8), ALU.add, ALU.mult),
                         reads=[modB, vecB], writes=[derB])
            for s in range(2):
                S.op("dve", TT(der[:, 0, s, 2, :], mod[:, 0, 16:24, s], V("outb", 0, 8), ALU.mult),
                     reads=[modB, vecB], writes=[derB])
            S.emit()

        def norm_mod(ph, tbs, l, kind, shift_j0, hT, hB, h32=None, h32B=None, after=None, nb=1):
            sq = [tile(ph, "sq", [128, 8, 512], BF16) for _ in range(nb)] * (3 - nb)
            sqB = [Buf() for _ in range(nb)] * (3 - nb)
            rs = [tile(ph, "rs", [128, 512], F32) for _ in range(2)]
            rsB = [Buf(), Buf()]
            t32 = [tile(ph, "t32", [128, 8, 512], F32) for _ in range(nb)] * (3 - nb) if h32 is None else h32
            t32B = [Buf() for _ in range(nb)] * (3 - nb) if h32 is None else h32B
            for i, tb in enumerate(tbs):
                t0, n = TBS[tb]
                s = 0 if tb < 4 else 1
                q, qB = sq[i % 2], sqB[i % 2]
                r, rB = rs[i % 2], rsB[i % 2]
                t, tB = t32[i % 2], t32B[i % 2]
                pp, ppB = PS[i % 2], PB[i % 2]
                xr = [xB[k][tb] for k in range(8)]
                S.op("act", ACT(q[:, :, :n], XH["t"][:, :, t0:t0 + n], AF.Square), reads=xr, writes=[qB])
                S.op("pe", MMS([(pp[:, :n], ones_b[:], q[:, k, :n], k == 0, k == 7) for k in range(8)]),
                     reads=[qB, cB], writes=[ppB])
                S.op("act", ACT(r[:, :n], pp[:, :n], AF.Ln, scale=1.0 / D, bias=V("eps")), reads=[ppB, vecB], writes=[rB])
                S.op("act", ACT(r[:, :n], r[:, :n], AF.Exp, scale=-0.5), reads=[rB], writes=[rB])
                for k in range(8):
                    S.op("dve", TT(t[:, k, :n], XH["t"][:, k, t0:t0 + n], r[:, :n], ALU.mult), reads=[xB[k][tb], rB], writes=[tB])
                for k in range(8):
                    if h32 is None:
                        S.op("act", ACT(hT[:, k, t0:t0 + n], t[:, k, :n], AF.Identity, scale=der[:, l, s, kind, k:k + 1],
                                        bias=mod[:, l, shift_j0 + k, s:s + 1]), reads=[tB, derB, modB], writes=[hB[tb]])
                    else:
                        S.op("act", ACT(t[:, k, :n], t[:, k, :n], AF.Identity, scale=der[:, l, s, kind, k:k + 1],
                                        bias=mod[:, l, shift_j0 + k, s:s + 1]), reads=[tB, derB, modB], writes=[tB])
                if h32 is not None:
                    S.op("pool", CP(hT[:, :, t0:t0 + n], t[:, :, :n]), reads=[tB], writes=[hB[tb]])
                    after(i, tb, t, tB)

        def filter_phase(Lf, which):
            NT = Lf // 128
            N2 = 2 * Lf
            with contextlib.ExitStack() as ph:
                zp = tile(ph, "zp", [17, Lf], F32)
                w0 = tile(ph, "w0", [17, 64], F32)
                wi = tile(ph, "wi", [64, 2, 64], F32)
                wout = tile(ph, "wout", [64, 2 * D], F32)
                dbc = tile(ph, "dbc", [128, D], F32)
                fbr = tile(ph, "fbr", [1, D], F32)
                ldB = Buf()
                S.dma("sp", zp[:], zpos_d[Lf], writes=[ldB])
                S.dma("sp", w0[:], fw0_d, writes=[ldB])
                S.dma("sp", wi[:], fwi_d.rearrange("n k m -> k n m"), writes=[ldB])
                S.dma("sp", wout[:], fwout_d, writes=[ldB])
                S.dma("sp", dbc[:], dbc_d, writes=[ldB])
                S.dma("sp", fbr[:], fbr_d, writes=[ldB])
                fsv = tile(ph, "fsv", [64, 4], F32)
                fsB = Buf()
                S.op("dve", TS(fsv[:, 0:1], V64("ffreq"), 1.0 / TWO_PI, None, ALU.mult), reads=[vecB], writes=[fsB])
                for i, nm in enumerate(["fb0", "fbi0", "fbi1"]):
                    S.op("dve", TS(fsv[:, i + 1:i + 2], V64(nm), fsv[:, 0:1], 16.0, ALU.mult, ALU.add), reads=[vecB, fsB], writes=[fsB])
                hA = tile(ph, "hA", [64, Lf], F32)
                hBt = tile(ph, "hBt", [64, Lf], F32)
                hbuf = [Buf(), Buf()]
                yt = [tile(ph, "yt", [64, 512], F32) for _ in range(2)]
                qi = [tile(ph, "qi", [64, 512], I32) for _ in range(2)]
                ytB = [Buf(), Buf()]
                blocks = [(t0, min(512, Lf - t0)) for t0 in range(0, Lf, 512)]
                it = 0
                for layer in range(3):
                    lhsT = w0[:, :] if layer == 0 else wi[:, layer - 1, :]
                    src, srcB = (zp, ldB) if layer == 0 else ((hA, hbuf[0]) if layer == 1 else (hBt, hbuf[1]))
                    dst, dstB = (hA, hbuf[0]) if layer in (0, 2) else (hBt, hbuf[1])
                    for (t0, n) in blocks:
                        pp, ppB = PS[it % 2], PB[it % 2]
                        y, q, yB = yt[it % 2], qi[it % 2], ytB[it % 2]
                        it += 1
                        S.op("pe", MMS([(pp[0:64, :n], lhsT, src[:, t0:t0 + n], True, True)]), reads=[ldB, srcB], writes=[ppB])
                        S.op("dve", TS(y[:, :n], pp[0:64, :n], fsv[:, 0:1], fsv[:, layer + 1:layer + 2], ALU.mult, ALU.add),
                             reads=[ppB, fsB], writes=[yB])
                        S.op("dve", CP(q[:, :n], y[:, :n]), reads=[yB], writes=[yB])
                        S.op("dve", TT(y[:, :n], y[:, :n], q[:, :n], ALU.subtract), reads=[yB], writes=[yB])
                        S.op("act", ACT(dst[:, t0:t0 + n], y[:, :n], AF.Sin, scale=TWO_PI), reads=[yB], writes=[dstB])
                h3, h3B = hA, hbuf[0]
                ks = tile(ph, "ks", [128, NT, D], BF16)
                kd = tile(ph, "kd", [128, NT, D], BF16)
                ksB = [Buf() for _ in range(NT)]
                acc = tile(ph, "acc", [128, D], F32)
                accB = Buf()
                ks0 = tile(ph, "ks0", [1, D], F32)
                ks0B = Buf()
                S.op("pool", MSET(acc[:], 0.0), writes=[accB])
                dec = [tile(ph, "dec", [128, D], F32) for _ in range(2)]
                kfb = [tile(ph, "kfb", [128, 2 * D], F32) for _ in range(2)]
                decB = [Buf(), Buf()]
                kfbB = [Buf(), Buf()]
                kab = tile(ph, "kab", [128, 2 * D], F32)
                kabB = Buf()
                tn = "tnx" if Lf == L else "tnc"
                adaW = WS(ph, "adaw", [128, 8, 256], nst=2, nbf=2) if Lf == L else None
                for tc in range(NT):
                    if adaW is not None:
                        for b3 in range(3):
                            ada_block(adaW, 3 * tc + b3)
                    de, deB = dec[tc % 2], decB[tc % 2]
                    kk, kkB = kfb[tc % 2], kfbB[tc % 2]
                    pk = [PS[(tc % 2) * 3 + i] for i in range(3)] + [PS[6]]
                    pkB = [PB[(tc % 2) * 3 + i] for i in range(3)] + [PB[6]]
                    for qd in range(4):
                        S.op("pe", MMS([(pk[qd][:, :], h3[:, tc * 128:(tc + 1) * 128], wout[:, qd * 512:(qd + 1) * 512], True, True)]),
                             reads=[h3B, ldB], writes=[pkB[qd]])
                    S.op("act", ACT(de[:], dbc[:], AF.Exp, scale=V(tn, tc)), reads=[ldB, vecB], writes=[deB])
                    for qd in range(4):
                        S.op("dve", TT(kk[:, qd * 512:(qd + 1) * 512], pk[qd][:, :], de[:, (qd % 2) * 512:(qd % 2 + 1) * 512], ALU.mult),
                             reads=[pkB[qd], deB], writes=[kkB])
                    if tc == 0:
                        S.op("dve", MSET(kk[0:1, D:2 * D], 0.0), reads=[kkB], writes=[kkB])
                    S.op("act", ACT(kab[:], kk[:], AF.Abs), reads=[kkB], writes=[kabB])
                    S.op("dve", TT(acc[:], acc[:], kab[:, 0:D], ALU.add), reads=[kabB, accB], writes=[accB])
                    S.op("dve", TT(acc[:], acc[:], kab[:, D:2 * D], ALU.add), reads=[kabB, accB], writes=[accB])
                    S.op("pool", TT(ks[:, tc, :], kk[:, 0:D], kk[:, D:2 * D], ALU.add), reads=[kkB], writes=[ksB[tc]])
                    S.op("pool", TT(kd[:, tc, :], kk[:, D:2 * D], kk[:, 0:D], ALU.subtract), reads=[kkB], writes=[ksB[tc]])
                    if tc == 0:
                        S.op("pool", TT(ks0[:], kk[0:1, 0:D], kk[0:1, D:2 * D], ALU.add), reads=[kkB], writes=[ks0B])
                pn, pnB = PS[0], PB[0]
                S.op("pe", MMS([(pn[:, j:j + 1], acc[:, j * 128:(j + 1) * 128], ones_f[:, 0:1], True, True) for j in range(8)]),
                     reads=[accB, cB], writes=[pnB])
                S.op("dve", lambda e: e.reciprocal(out=invn[:, which, :], in_=pn[:, 0:8]), reads=[pnB], writes=[invnB])
                S.op("dve", TS(invn[:, which, :], invn[:, which, :], 2.0 / N2, None, ALU.mult), reads=[invnB], writes=[invnB])
                for hf in range(2):
                    pr, prB = PS[1 + hf], PB[1 + hf]
                    S.op("pe", MMS([(pr[0:1, :], ones_f[:, 0:1], acc[:, hf * 512:(hf + 1) * 512], True, True)]),
                         reads=[accB, cB], writes=[prB])
                    S.op("dve", TT(dec[0][0:1, hf * 512:(hf + 1) * 512], pr[0:1, :], fbr[:, hf * 512:(hf + 1) * 512], ALU.mult),
                         reads=[prB, ldB], writes=[decB[0]])
                S.op("dve", TT(ks[0:1, 0, :], dec[0][0:1, :], ks0[:], ALU.add), reads=[decB[0], ks0B], writes=[ksB[0]])
                tab = [tile(ph, "tab", [128, 2, NT, 128], BF16) for _ in range(2)]
                tabB = [Buf(), Buf()]
                pqt = [tile(ph, "pqt", [128, 2, D], BF16) for _ in range(2)]
                pqB = [Buf(), Buf()]
                S.dma("sp", tab[0][:], dftf_d[Lf][0], writes=[tabB[0]])
                for fc in range(NT):
                    tb_, tbB = tab[fc % 2], tabB[fc % 2]
                    if fc + 1 < NT:
                        S.dma("sp", tab[(fc + 1) % 2][:], dftf_d[Lf][fc + 1], writes=[tabB[(fc + 1) % 2]])
                    pq, pqb = pqt[fc % 2], pqB[fc % 2]
                    for cb in range(2):
                        for cs in range(2):
                            pi = (fc % 2) * 3 + cs if cb == 0 else ((fc % 2) * 3 + 2 if cs == 0 else 6)
                            src = ks if cs == 0 else kd
                            S.op("pe", MMS([(PS[pi][:, :], tb_[:, cs, sc, :], src[:, sc, cb * 512:(cb + 1) * 512], sc == 0, sc == NT - 1)
                                            for sc in range(NT)]), reads=[tbB] + ksB, writes=[PB[pi]])
                            eng = "act" if cs == 0 else "dve"
                            fn = ACT(pq[:, cs, cb * 512:(cb + 1) * 512], PS[pi][:, :], AF.Copy) if cs == 0 else CP(pq[:, cs, cb * 512:(cb + 1) * 512], PS[pi][:, :])
                            S.op(eng, fn, reads=[PB[pi]], writes=[pqb])
                    S.dma("sp", pq_d[Lf][fc], pq[:], reads=[pqb])
                S.emit()

        if stop_after >= 1:
            filter_phase(L, 0)
            ada_derived()
            filter_phase(LC, 1)

        if stop_after >= 2:
            with contextlib.ExitStack() as msc:
                v_tok = tile(msc, "vtok", [128, 18, D], BF16)
                vtB = [Buf() for _ in range(18)]
                with contextlib.ExitStack() as hsc:
                    hT = tile(hsc, "hT", [128, 8, NTOK], BF16)
                    hB = [Buf() for _ in TBS]
                    with contextlib.ExitStack() as ph:
                        XH["t"] = tile(ph, "xT", [128, 8, NTOK], F32)
                        for k in range(8):
                            S.dma("sp", XH["t"][:, k, 0:L], xT_d[k * 128:(k + 1) * 128, :], writes=xB[k][0:4])
                            S.dma("sp", XH["t"][:, k, L:NTOK], cT_d[k * 128:(k + 1) * 128, :], writes=[xB[k][4]])
                        norm_mod(ph, range(5), 0, 0, 0, hT, hB)
                        for k in range(8):
                            S.dma("sp", xs_d[k], XH["t"][:, k, :], reads=xB[k])
                        S.emit()
                        XH["t"] = None
                    with contextlib.ExitStack() as ph:
                        wst = WS(ph, "inw", [128, 8, 128], nst=4, nbf=4)
                        z = [tile(ph, "z", [128, NTOK + 4], F32) for _ in range(2)]
                        zB = [Buf(), Buf()]
                        for zz, zb in zip(z, zB):
                            S.op("pool", MSET(zz[:], 0.0), writes=[zb])
                        cA = tile(ph, "cA", [128, NTOK], F32)
                        cBt = tile(ph, "cBt", [128, NTOK], F32)
                        cAB, cBB = Buf(), Buf()
                        x0o = [tile(ph, "x0o", [128, NTOK], BF16) for _ in range(2)]
                        x0B = [Buf(), Buf()]
                        vTt = [tile(ph, "vTt", [128, NTOK], BF16) for _ in range(2)]
                        vTB = [Buf(), Buf()]
                        zi = 0
                        SEG = [(1, 0, L), (L + 3, L, LC)]

                        def proj_conv(m, out_ap_fn, outB):
                            nonlocal zi
                            wt, wb = wst.finish(wq_pref.pop(0))
                            if roles:
                                mm_ = roles.pop(0)
                                wq_pref.append(wst.issue(in_w_d[:, mm_ * 128:(mm_ + 1) * 128].rearrange("(k p) n -> p k n", p=128)))
                            zz, zb = z[zi % 2], zB[zi % 2]
                            zi += 1
                            for tb, (t0, n) in enumerate(TBS):
                                pp, ppB = PS[tb % 4], PB[tb % 4]
                                S.op("pe", MMS([(pp[:, :n], wt[:, k, :], hT[:, k, t0:t0 + n], k == 0, k == 7) for k in range(8)]),
                                     reads=[wb, hB[tb]], writes=[ppB])
                                zc = (1 + t0) if tb < 4 else (L + 3)
                                S.op("act", ACT(zz[:, zc:zc + n], pp[:, :n], AF.Identity, bias=V("inb", m)), reads=[ppB, vecB], writes=[zb])
                            for (zc, t0, n) in SEG:
                                o = out_ap_fn(t0, n)
                                S.op("dve", TS(tmpc[:, t0:t0 + n], zz[:, zc:zc + n], V("scw1", m), V("scb", m), ALU.mult, ALU.add),
                                     reads=[zb, vecB], writes=[tmpcB])
                                S.op("dve", STT(tmpc[:, t0:t0 + n], zz[:, zc - 1:zc - 1 + n], V("scw0", m), tmpc[:, t0:t0 + n], ALU.mult, ALU.add),
                                     reads=[zb, vecB, tmpcB], writes=[tmpcB])
                                S.op("dve", STT(o, zz[:, zc + 1:zc + 1 + n], V("scw2", m), tmpc[:, t0:t0 + n], ALU.mult, ALU.add),
                                     reads=[zb, vecB, tmpcB], writes=[outB])

                        tmpc = tile(ph, "tmpc", [128, NTOK], F32)
                        tmpcB = Buf()
                        roles = [m_ for j in range(8) for m_ in (j, 8 + j, 16 + j)]
                        wq_pref = []
                        for _ in range(3):
                            mm_ = roles.pop(0)
                            wq_pref.append(wst.issue(in_w_d[:, mm_ * 128:(mm_ + 1) * 128].rearrange("(k p) n -> p k n", p=128)))
                        for j in range(8):
                            xo, xoB = x0o[j % 2], x0B[j % 2]
                            proj_conv(j, lambda t0, n: xo[:, t0:t0 + n], xoB)
                            proj_conv(8 + j, lambda t0, n: cA[:, t0:t0 + n], cAB)
                            proj_conv(16 + j, lambda t0, n: cBt[:, t0:t0 + n], cBB)
                            S.dma("sp", x0_d[j], xo[:], reads=[xoB])
                            vt, vB = vTt[j % 2], vTB[j % 2]
                            S.op("dve", TT(vt[:], cA[:], cBt[:], ALU.mult), reads=[cAB, cBB], writes=[vB])
                            for b3 in range(3):
                                bank = PSB if b3 % 2 == 0 else PS6b
                                bB_ = PBB[b3 % 2]

                                def trs(e, bank=bank, b3=b3, vt=vt):
                                    ins = None
                                    for i6 in range(6):
                                        tc = b3 * 6 + i6
                                        ins = e.transpose(bank[:, i6 * 128:(i6 + 1) * 128], vt[:, tc * 128:(tc + 1) * 128], ident_b[:])
                                    return ins
                                S.op("pe", trs, reads=[vB, cB], writes=[bB_])
                                S.op("act", ACT(v_tok[:, b3 * 6:(b3 + 1) * 6, j * 128:(j + 1) * 128],
                                                bank[:, 0:768].rearrange("p (a b) -> p a b", a=6), AF.Copy),
                                     reads=[bB_], writes=[vtB[b3 * 6 + i6] for i6 in range(6)])
                        S.emit()

                if stop_after >= 3:
                    def longconv(Lf, which, tc0, tok0):
                        NT = Lf // 128
                        with contextlib.ExitStack() as ph:
                            Yr = tile(ph, "Yr", [128, NT, 512], BF16)
                            Yi = tile(ph, "Yi", [128, NT, 512], BF16)
                            YB = [Buf() for _ in range(NT)]
                            tab = [tile(ph, "ftab", [128, 2, NT, 128], BF16) for _ in range(2)]
                            tabB = [Buf(), Buf()]
                            pqt = [tile(ph, "pq", [128, 2, 512], BF16) for _ in range(2)]
                            pqB = [Buf(), Buf()]
                            tm = [tile(ph, "tm", [128, 4, 512], F32) for _ in range(2)]
                            tmB = [Buf(), Buf()]
                            itab = [tile(ph, "itab", [128, 2, 512], BF16) for _ in range(3)]
                            itB = [Buf() for _ in range(3)]
                            x0t = [tile(ph, "x0t", [128, 512], BF16) for _ in range(8)]
                            x0B = [Buf() for _ in range(8)]
                            ut = [tile(ph, "ut", [128, 512], BF16) for _ in range(4)]
                            utB = [Buf() for _ in range(4)]
                            blocks = [(t0, min(512, Lf - t0)) for t0 in range(0, Lf, 512)]
                            ii = 0
                            xi = 0
                            ustores = []
                            for cb in range(2):
                                for fc in range(NT):
                                    tb_, tbB = tab[fc % 2], tabB[fc % 2]
                                    S.dma("sp", tb_[:], dftf_d[Lf][fc], writes=[tbB])
                                    pq, pqb = pqt[fc % 2], pqB[fc % 2]
                                    S.dma("sp", pq[:], pq_d[Lf][fc][:, :, cb * 512:(cb + 1) * 512], writes=[pqb])
                                    pA, pAB = PS[(fc % 2) * 2], PB[(fc % 2) * 2]
                                    pBm, pBB = PS[(fc % 2) * 2 + 1], PB[(fc % 2) * 2 + 1]
                                    vr = [vtB[tc0 + sc] for sc in range(NT)]
                                    S.op("pe", MMS([(pA[:, :], tb_[:, 0, sc, :], v_tok[:, tc0 + sc, cb * 512:(cb + 1) * 512], sc == 0, sc == NT - 1)
                                                    for sc in range(NT)]), reads=[tbB] + vr, writes=[pAB])
                                    S.op("pe", MMS([(pBm[:, :], tb_[:, 1, sc, :], v_tok[:, tc0 + sc, cb * 512:(cb + 1) * 512], sc == 0, sc == NT - 1)
                                                    for sc in range(NT)]), reads=[tbB] + vr, writes=[pBB])
                                    t, tB = tm[fc % 2], tmB[fc % 2]
                                    S.op("dve", TT(t[:, 0, :], pA[:, :], pq[:, 0, :], ALU.mult), reads=[pAB, pqb], writes=[tB])
                                    S.op("dve", TT(t[:, 1, :], pBm[:, :], pq[:, 1, :], ALU.mult), reads=[pBB, pqb], writes=[tB])
                                    S.op("dve", TT(t[:, 2, :], pBm[:, :], pq[:, 0, :], ALU.mult), reads=[pBB, pqb], writes=[tB])
                                    S.op("dve", TT(t[:, 3, :], pA[:, :], pq[:, 1, :], ALU.mult), reads=[pAB, pqb], writes=[tB])
                                    S.op("pool", TT(Yr[:, fc, :], t[:, 0, :], t[:, 1, :], ALU.add), reads=[tB], writes=[YB[fc]])
                                    S.op("pool", TT(Yi[:, fc, :], t[:, 2, :], t[:, 3, :], ALU.subtract), reads=[tB], writes=[YB[fc]])
                                for (t0, n) in blocks:
                                    for j in range(4):
                                        S.dma("sp", x0t[(xi + j) % 8][:, :n], x0_d[cb * 4 + j][:, tok0 + t0:tok0 + t0 + n], writes=[x0B[(xi + j) % 8]])
                                    for fc in range(NT):
                                        itb, itb_B = itab[ii % 3], itB[ii % 3]
                                        ii += 1
                                        S.dma("sp", itb[:, :, :n], dfti_d[Lf][fc][:, :, t0:t0 + n], writes=[itb_B])
                                        if fc == min(2, NT - 1):
                                            while ustores:
                                                o_, i_, b_u = ustores.pop(0)
                                                S.dma("sp", o_, i_, reads=[b_u])
                                        for j in range(4):
                                            S.op("pe", MMS([(PS[j][:, :n], Yr[:, fc, j * 128:(j + 1) * 128], itb[:, 0, :n], fc == 0, False),
                                                            (PS[j][:, :n], Yi[:, fc, j * 128:(j + 1) * 128], itb[:, 1, :n], False, fc == NT - 1)]),
                                                 reads=[YB[fc], itb_B], writes=[PB[j]])
                                    for j in range(4):
                                        jj = cb * 4 + j
                                        xt_, xtB = x0t[xi % 8], x0B[xi % 8]
                                        uu, uB = ut[xi % 4], utB[xi % 4]
                                        xi += 1
                                        S.op("dve", STT(uu[:, :n], PS[j][:, :n], invn[:, which, jj:jj + 1], xt_[:, :n], ALU.mult, ALU.mult),
                                             reads=[PB[j], invnB, xtB], writes=[uB])
                                        ustores.append((u_d[jj][:, tok0 + t0:tok0 + t0 + n], uu[:, :n], uB))
                            while ustores:
                                o_, i_, b_u = ustores.pop(0)
                                S.dma("sp", o_, i_, reads=[b_u])
                            S.emit()

                    longconv(L, 0, 0, 0)
                    longconv(LC, 1, 16, L)

            XH["t"] = tile(gs, "xT2", [128, 8, NTOK], F32)
            for k in range(8):
                S.dma("sp", XH["t"][:, k, :], xs_d[k], writes=xB[k])
            if stop_after >= 3:
                with contextlib.ExitStack() as ph:
                    uT = tile(ph, "uT", [128, 8, NTOK], BF16)
                    uB = Buf()
                    for k in range(8):
                        S.dma("sp", uT[:, k, :], u_d[k], writes=[uB])
                    wst = WS(ph, "outw", [128, 8, 128])
                    tmp = [tile(ph, "otmp", [128, 512], F32) for _ in range(2)]
                    tmpB = [Buf(), Buf()]
                    it = 0
                    for m in range(8):
                        wt, wb = wst.load(out_w_d[:, m * 128:(m + 1) * 128].rearrange("(k p) n -> p k n", p=128))
                        for tb, (t0, n) in enumerate(TBS):
                            s = 0 if tb < 4 else 1
                            pp, ppB = PS[it % 4], PB[it % 4]
                            tt, ttB = tmp[it % 2], tmpB[it % 2]
                            it += 1
                            S.op("pe", MMS([(pp[:, :n], wt[:, k, :], uT[:, k, t0:t0 + n], k == 0, k == 7) for k in range(8)]),
                                 reads=[wb, uB], writes=[ppB])
                            S.op("act", ACT(tt[:, :n], pp[:, :n], AF.Identity, scale=mod[:, 0, 16 + m, s:s + 1], bias=der[:, 0, s, 2, m:m + 1]),
                                 reads=[ppB, modB, derB], writes=[ttB])
                            S.op("dve", TT(XH["t"][:, m, t0:t0 + n], XH["t"][:, m, t0:t0 + n], tt[:, :n], ALU.add), reads=[ttB, xB[m][tb]], writes=[xB[m][tb]])
                    S.emit()
        if stop_after == 3:
            dump()

        def ffn_alloc(ph, ntok=NTOK):
            return dict(w1s=WS(ph, "w1", [128, 8, 128]), w3s=WS(ph, "w3", [128, 8, 128]), w2s=WS(ph, "w2", [128, 7, 128]),
                        ug=tile(ph, "ug", [128, 7, ntok], BF16), ugB=[[Buf() for _ in TBS] for _ in range(7)],
                        sa=[tile(ph, "sa", [128, 512], F32) for _ in range(2)], saB=[Buf(), Buf()], it=[0])

        def ffn(R, hT, hB, tbs, w1d, w3d, w2d, gate_fn):
            w1s, w3s, w2s, ug, ugB, sa, saB = R["w1s"], R["w3s"], R["w2s"], R["ug"], R["ugB"], R["sa"], R["saB"]
            it = R["it"][0]
            for g in range(4):
                for mi in range(7):
                    m = g * 7 + mi
                    a, aB = w1s.load(w1d[:, m * 128:(m + 1) * 128].rearrange("(k p) n -> p k n", p=128))
                    b, bB = w3s.load(w3d[:, m * 128:(m + 1) * 128].rearrange("(k p) n -> p k n", p=128))
                    for tb in tbs:
                        t0, n = TBS[tb]
                        pa, paB = PS[(it % 2) * 2], PB[(it % 2) * 2]
                        pb, pbB = PS[(it % 2) * 2 + 1], PB[(it % 2) * 2 + 1]
                        s_, sB_ = sa[it % 2], saB[it % 2]
                        it += 1
                        S.op("pe", MMS([(pa[:, :n], a[:, k, :], hT[:, k, t0:t0 + n], k == 0, k == 7) for k in range(8)]),
                             reads=[aB, hB[tb]], writes=[paB])
                        S.op("pe", MMS([(pb[:, :n], b[:, k, :], hT[:, k, t0:t0 + n], k == 0, k == 7) for k in range(8)]),
                             reads=[bB, hB[tb]], writes=[pbB])
                        S.op("act", ACT(s_[:, :n], pa[:, :n], AF.Silu), reads=[paB], writes=[sB_])
                        S.op("dve", TT(ug[:, mi, t0:t0 + n], s_[:, :n], pb[:, :n], ALU.mult), reads=[sB_, pbB], writes=[ugB[mi][tb]])
                for m in range(8):
                    w, wB = w2s.load(w2d[g * 896:(g + 1) * 896, m * 128:(m + 1) * 128].rearrange("(k p) n -> p k n", p=128))
                    for tb in tbs:
                        t0, n = TBS[tb]
                        pp, ppB = PS[4 + it % 3], PB[4 + it % 3]
                        it += 1
                        S.op("pe", MMS([(pp[:, :n], w[:, mi, :], ug[:, mi, t0:t0 + n], mi == 0, mi == 6) for mi in range(7)]),
                             reads=[wB] + [ugB[mi][tb] for mi in range(7)], writes=[ppB])
                        gate_fn(m, tb, pp, ppB, t0, n)
            R["it"][0] = it

        if stop_after >= 4:
            with contextlib.ExitStack() as hsc:
                hT = tile(hsc, "hT", [128, 8, NTOK], BF16)
                hB = [Buf() for _ in TBS]
                with contextlib.ExitStack() as ph:
                    norm_mod(ph, range(5), 0, 1, 24, hT, hB, nb=2)
                    S.emit()
                with contextlib.ExitStack() as ph:
                    def gate0(m, tb, pp, ppB, t0, n):
                        s = 0 if tb < 4 else 1
                        S.op("dve", STT(XH["t"][:, m, t0:t0 + n], pp[:, :n], mod[:, 0, 40 + m, s:s + 1], XH["t"][:, m, t0:t0 + n], ALU.mult, ALU.add),
                             reads=[ppB, modB, xB[m][tb]], writes=[xB[m][tb]])
                    ffn(ffn_alloc(ph), hT, hB, range(5), w1_d, w3_d, w2_d, gate0)
                    S.emit()
        if stop_after == 4:
            dump()

        if stop_after >= 5:
            v_d = dscr("v_s", [128, 18, 16, 65], BF16)
            with contextlib.ExitStack() as asc:
                ckn = tile(asc, "ckn", [128, 2, NTOK], BF16)
                cknB = [Buf() for _ in TBS]
                with contextlib.ExitStack() as hsc:
                    hT = tile(hsc, "hT", [128, 8, NTOK], BF16)
                    hB = [Buf() for _ in TBS]
                    with contextlib.ExitStack() as ph:
                        norm_mod(ph, range(5), 1, 0, 0, hT, hB, nb=2)
                        S.emit()
                    with contextlib.ExitStack() as ph:
                        ropeC = tile(ph, "ropeC", [96, L], BF16)
                        ropeS = tile(ph, "ropeS", [96, L], BF16)
                        rpB = Buf()
                        S.dma("sp", ropeC[64:96, :], ropeC_d, writes=[rpB])
                        S.dma("sp", ropeS[64:96, :], ropeS_d, writes=[rpB])
                        wqa = WS(ph, "wqa", [128, 8, 384], nst=1, nbf=1)
                        wa, waB = wqa.load(wqa_d.rearrange("(k p) n -> p k n", p=128))
                        qn = tile(ph, "qn", [128, 3, L], BF16)
                        qnB = [Buf() for _ in range(4)]
                        sq = [tile(ph, "qsq", [128, 3, 512], BF16) for _ in range(2)]
                        sqB = [Buf(), Buf()]
                        rs = [tile(ph, "qrs", [128, 512], F32) for _ in range(2)]
                        rsB = [Buf(), Buf()]
                        t3 = [tile(ph, "qt3", [128, 3, 512], F32) for _ in range(2)]
                        t3B = [Buf(), Buf()]
                        for tb in range(4):
                            t0, n = TBS[tb]
                            for c in range(3):
                                S.op("pe", MMS([(PS[c][:, :], wa[:, k, c * 128:(c + 1) * 128], hT[:, k, t0:t0 + n], k == 0, k == 7) for k in range(8)]),
                                     reads=[waB, hB[tb]], writes=[PB[c]])
                                S.op("act", ACT(sq[tb % 2][:, c, :], PS[c][:, :], AF.Square), reads=[PB[c]], writes=[sqB[tb % 2]])
                            S.op("pe", MMS([(PS[3][:, :], ones_b[:], sq[tb % 2][:, c, :], c == 0, c == 2) for c in range(3)]),
                                 reads=[sqB[tb % 2], cB], writes=[PB[3]])
                            r, rB = rs[tb % 2], rsB[tb % 2]
                            S.op("act", ACT(r[:], PS[3][:, :], AF.Ln, scale=1.0 / 384, bias=V("eps")), reads=[PB[3], vecB], writes=[rB])
                            S.op("act", ACT(r[:], r[:], AF.Exp, scale=-0.5), reads=[rB], writes=[rB])
                            for c in range(3):
                                S.op("dve", TT(t3[tb % 2][:, c, :], PS[c][:, :], r[:], ALU.mult), reads=[PB[c], rB], writes=[t3B[tb % 2]])
                                S.op("act", ACT(qn[:, c, t0:t0 + n], t3[tb % 2][:, c, :], AF.Identity, scale=V("qnorm", c)),
                                     reads=[t3B[tb % 2], vecB], writes=[qnB[tb]])
                        wqb = WS(ph, "wqb", [128, 3, 128])
                        ropeCS = tile(ph, "ropeCS", [128, L], BF16)
                        shm = tile(ph, "shm", [128, 96], BF16)
                        S.dma("sp", ropeCS[:], ropeCS_d, writes=[rpB])
                        S.dma("sp", shm[:], shift_d, writes=[rpB])
                        tT = [tile(ph, "tT", [128, 512], BF16) for _ in range(3)]
                        tTB = [Buf() for _ in range(3)]
                        qo = [tile(ph, "qo", [96, 512], BF16) for _ in range(3)]
                        qoB = [Buf() for _ in range(3)]
                        it = 0
                        qpref = [wqb.issue(wqb2_d[:, 0, :].rearrange("(k p) n -> p k n", p=128))]
                        for h in range(16):
                            w, wB = wqb.finish(qpref.pop(0))
                            if h + 1 < 16:
                                qpref.append(wqb.issue(wqb2_d[:, h + 1, :].rearrange("(k p) n -> p k n", p=128)))
                            for tb in range(4):
                                t0, n = TBS[tb]
                                i2 = it % 3
                                it += 1
                                pq_, pqB_ = PS[i2], PB[i2]
                                p2_, p2B_ = PS[3 + i2], PB[3 + i2]
                                S.op("pe", MMS([(pq_[:, :], w[:, c, :], qn[:, c, t0:t0 + n], c == 0, c == 2) for c in range(3)]),
                                     reads=[wB, qnB[tb]], writes=[pqB_])
                                S.op("dve", TT(tT[i2][:], pq_[:, :], ropeCS[:, t0:t0 + n], ALU.mult), reads=[pqB_, rpB], writes=[tTB[i2]])
                                S.op("pe", MMS([(p2_[0:96, :], shm[:], tT[i2][:], True, True)]), reads=[rpB, tTB[i2]], writes=[p2B_])
                                S.op("act", ACT(qo[i2][:], p2_[0:96, :], AF.Copy), reads=[p2B_], writes=[qoB[i2]])
                                S.dma("sp", q_d[h][:, t0:t0 + n], qo[i2][:], reads=[qoB[i2]])
                        S.emit()
                    with contextlib.ExitStack() as ph:
                        ropeC = tile(ph, "ropeC", [96, L], BF16)
                        ropeS = tile(ph, "ropeS", [96, L], BF16)
                        rpB = Buf()
                        S.dma("sp", ropeC[64:96, :], ropeC_d, writes=[rpB])
                        S.dma("sp", ropeS[64:96, :], ropeS_d, writes=[rpB])
                        wkva = WS(ph, "wkva", [128, 8, 256], nst=1, nbf=1)
                        wa, waB = wkva.load(wkva_d.rearrange("(k p) n -> p k n", p=128))
                        wkp = WS(ph, "wkp", [128, 8, 96], nst=2, nbf=2)
                        wp, wpB = wkp.load(wkpe_d.rearrange("(k p) n -> p k n", p=128))
                        wps, wpsB = wkp.load(wkpes_d.rearrange("(k p) n -> p k n", p=128))
                        kpe = tile(ph, "kpe", [96, NTOK], BF16)
                        kpeB = Buf()
                        sq = [tile(ph, "ksq", [128, 2, 512], BF16) for _ in range(2)]
                        sqB = [Buf(), Buf()]
                        rs = [tile(ph, "krs", [128, 512], F32) for _ in range(2)]
                        rsB = [Buf(), Buf()]
                        t3 = [tile(ph, "kt3", [128, 2, 512], F32) for _ in range(2)]
                        t3B = [Buf(), Buf()]
                        ka_t = [tile(ph, "ka_t", [96, 512], F32) for _ in range(2)]
                        kb_t = [tile(ph, "kb_t", [96, 512], F32) for _ in range(2)]
                        kaB, kbB = [Buf(), Buf()], [Buf(), Buf()]
                        for tb, (t0, n) in enumerate(TBS):
                            i2 = tb % 2
                            for c in range(2):
                                S.op("pe", MMS([(PS[c][:, :n], wa[:, k, c * 128:(c + 1) * 128], hT[:, k, t0:t0 + n], k == 0, k == 7) for k in range(8)]),
                                     reads=[waB, hB[tb]], writes=[PB[c]])
                                S.op("act", ACT(sq[i2][:, c, :n], PS[c][:, :n], AF.Square), reads=[PB[c]], writes=[sqB[i2]])
                            S.op("pe", MMS([(PS[2][:, :n], ones_b[:], sq[i2][:, c, :n], c == 0, c == 1) for c in range(2)]),
                                 reads=[sqB[i2], cB], writes=[PB[2]])
                            r, rB = rs[i2], rsB[i2]
                            S.op("act", ACT(r[:, :n], PS[2][:, :n], AF.Ln, scale=1.0 / 256, bias=V("eps")), reads=[PB[2], vecB], writes=[rB])
                            S.op("act", ACT(r[:, :n], r[:, :n], AF.Exp, scale=-0.5), reads=[rB], writes=[rB])
                            for c in range(2):
                                S.op("dve", TT(t3[i2][:, c, :n], PS[c][:, :n], r[:, :n], ALU.mult), reads=[PB[c], rB], writes=[t3B[i2]])
                                S.op("act", ACT(ckn[:, c, t0:t0 + n], t3[i2][:, c, :n], AF.Identity, scale=V("kvnorm", c)),
                                     reads=[t3B[i2], vecB], writes=[cknB[tb]])
                            S.op("pe", MMS([(PS[3][0:96, :n], wp[:, k, :], hT[:, k, t0:t0 + n], k == 0, k == 7) for k in range(8)]),
                                 reads=[wpB, hB[tb]], writes=[PB[3]])
                            if tb < 4:
                                S.op("pe", MMS([(PS[4][0:96, :n], wps[:, k, :], hT[:, k, t0:t0 + n], k == 0, k == 7) for k in range(8)]),
                                     reads=[wpsB, hB[tb]], writes=[PB[4]])
                                S.op("dve", TT(ka_t[i2][64:96, :n], PS[3][64:96, :n], ropeC[64:96, t0:t0 + n], ALU.mult), reads=[PB[3], rpB], writes=[kaB[i2]])
                                S.op("dve", TT(kb_t[i2][64:96, :n], PS[4][64:96, :n], ropeS[64:96, t0:t0 + n], ALU.mult), reads=[PB[4], rpB], writes=[kbB[i2]])
                                S.op("dve", TT(kpe[64:96, t0:t0 + n], ka_t[i2][64:96, :n], kb_t[i2][64:96, :n], ALU.add), reads=[kaB[i2], kbB[i2]], writes=[kpeB])
                            else:
                                S.op("act", ACT(kpe[64:96, t0:t0 + n], PS[3][64:96, :n], AF.Copy), reads=[PB[3]], writes=[kpeB])
                        for h in range(16):
                            S.dma("sp", k_d[h][64:96, :], kpe[64:96, :], reads=[kpeB])
                        wkb = WS(ph, "wkb", [128, 2, 64])
                        ko = [tile(ph, "ko", [64, NTOK], BF16) for _ in range(2)]
                        koB = [Buf(), Buf()]
                        it = 0
                        kpref = [wkb.issue(wkbn_d[:, 0, :].rearrange("(k p) n -> p k n", p=128))]
                        for h in range(16):
                            w, wB = wkb.finish(kpref.pop(0))
                            if h + 1 < 16:
                                kpref.append(wkb.issue(wkbn_d[:, h + 1, :].rearrange("(k p) n -> p k n", p=128)))
                            for tb, (t0, n) in enumerate(TBS):
                                pp, ppB = PS[it % 4], PB[it % 4]
                                it += 1
                                S.op("pe", MMS([(pp[0:64, :n], w[:, c, :], ckn[:, c, t0:t0 + n], c == 0, c == 1) for c in range(2)]),
                                     reads=[wB, cknB[tb]], writes=[ppB])
                                S.op("act" if tb % 2 == 0 else "dve",
                                     ACT(ko[h % 2][:, t0:t0 + n], pp[0:64, :n], AF.Copy) if tb % 2 == 0 else CP(ko[h % 2][:, t0:t0 + n], pp[0:64, :n]),
                                     reads=[ppB], writes=[koB[h % 2]])
                            S.dma("sp", k_d[h][0:64, :], ko[h % 2][:], reads=[koB[h % 2]])
                        S.emit()
                with contextlib.ExitStack() as ph:
                        V_all = tile(ph, "Vall", [128, 18, 16, 65], BF16)
                        VB = [Buf() for _ in range(18)]
                        wv = WS(ph, "wvb", [128, 2, 1024], nst=1, nbf=1)
                        wvt, wvB = wv.load(wvb_d.rearrange("(k p) n -> p k n", p=128))
                        for kc in range(18):
                            tb = min(kc // 4, 4)
                            S.op("pool", MSET(V_all[:, kc, :, 64:65], 1.0), writes=[VB[kc]])
                            for hf in range(2):
                                pp, ppB = PS[(kc * 2 + hf) % 4], PB[(kc * 2 + hf) % 4]
                                S.op("pe", MMS([(pp[:, :], ckn[:, c, kc * 128:(kc + 1) * 128], wvt[:, c, hf * 512:(hf + 1) * 512], c == 0, c == 1) for c in range(2)]),
                                     reads=[wvB, cknB[tb]], writes=[ppB])
                                S.op("act" if hf == 0 else "dve",
                                     (ACT if hf == 0 else (lambda o, i, f: CP(o, i)))(V_all[:, kc, hf * 8:(hf + 1) * 8, 0:64], pp[:, :].rearrange("p (h d) -> p h d", h=8), AF.Copy),
                                     reads=[ppB], writes=[VB[kc]])
                        S.dma("sp", v_d, V_all[:], reads=VB)
                        S.emit()
            if True:
                with contextlib.ExitStack() as ph:
                    V_all = tile(ph, "Vall2", [128, 18, 16, 65], BF16)
                    VB = [Buf() for _ in range(18)]
                    S.dma("sp", V_all[:], v_d, writes=VB)
                    KT = [tile(ph, "KT", [96, NTOK], BF16) for _ in range(2)]
                    QT = [tile(ph, "QT", [96, L], BF16) for _ in range(2)]
                    KTB, QTB = [Buf(), Buf()], [Buf(), Buf()]
                    PT = [tile(ph, "PT", [128, 512], BF16) for _ in range(3)]
                    PTB = [Buf() for _ in range(3)]
                    rd = [tile(ph, "rd", [65, 512], F32) for _ in range(2)]
                    rdB = [Buf(), Buf()]
                    bc = [tile(ph, "bc", [64, 512], F32) for _ in range(2)]
                    bcB = [Buf(), Buf()]
                    ao = [tile(ph, "ao", [64, L], BF16) for _ in range(2)]
                    aoB = [Buf(), Buf()]
                    scale = 1.0 / float(np.sqrt(96.0))
                    iters = [(h, qb, kc) for h in range(16) for qb in range(4) for kc in range(18)]
                    nit = len(iters)
                    pending = []

                    def load_head(h):
                        S.dma("sp", KT[h % 2][:], k_d[h], writes=[KTB[h % 2]])
                        S.dma("sp", QT[h % 2][:], q_d[h], writes=[QTB[h % 2]])

                    def finalize(g):
                        h, qb = g // 4, g % 4
                        po, poB = PS[4 + g % 2], PB[4 + g % 2]
                        r_, rB_ = rd[g % 2], rdB[g % 2]
                        b_, bB_ = bc[g % 2], bcB[g % 2]
                        S.op("pe", MMS([(PS[6][0:64, :], ones_f[64:65, 0:64], r_[64:65, :], True, True)]), reads=[rB_, cB], writes=[PB[6]])
                        S.op("dve", CP(b_[:], PS[6][0:64, :]), reads=[PB[6]], writes=[bB_])
                        S.op("dve", TT(ao[h % 2][:, qb * 512:(qb + 1) * 512], po[0:64, :], b_[:], ALU.mult), reads=[poB, bB_], writes=[aoB[h % 2]])
                        if qb == 3:
                            S.dma("sp", ao_d[h], ao[h % 2][:], reads=[aoB[h % 2]])

                    load_head(0)
                    LOOK = 2
                    for j in range(nit + LOOK):
                        if j < nit:
                            h, qb, kc = iters[j]
                            if qb == 0 and kc == 0 and h + 1 < 16:
                                load_head(h + 1)
                            kt, ktB, qt, qtB = KT[h % 2], KTB[h % 2], QT[h % 2], QTB[h % 2]
                            pS, pSB, pt, ptB = PS[j % 3], PB[j % 3], PT[j % 3], PTB[j % 3]
                            S.op("pe", MMS([(pS[:, :], kt[:, kc * 128:(kc + 1) * 128], qt[:, qb * 512:(qb + 1) * 512], True, True)]),
                                 reads=[ktB, qtB], writes=[pSB])
                            S.op("act", ACT(pt[:], pS[:, :], AF.Exp, scale=scale), reads=[pSB], writes=[ptB])
                        jj = j - LOOK
                        if jj >= 0:
                            h, qb, kc = iters[jj]
                            g = h * 4 + qb
                            po, poB = PS[4 + g % 2], PB[4 + g % 2]
                            pt, ptB = PT[jj % 3], PTB[jj % 3]
                            S.op("pe", MMS([(po[0:65, :], V_all[:, kc, h, :], pt[:], kc == 0, kc == 17)]), reads=[VB[kc], ptB], writes=[poB])
                            if kc == 17:
                                r_, rB_ = rd[g % 2], rdB[g % 2]
                                S.op("dve", lambda e, r_=r_, po=po: e.reciprocal(out=r_[64:65, :], in_=po[64:65, :]), reads=[poB], writes=[rB_])
                                pending.append((j + 9, g))
                        while pending and pending[0][0] <= j:
                            finalize(pending.pop(0)[1])
                    while pending:
                        finalize(pending.pop(0)[1])
                    S.emit()
            with contextlib.ExitStack() as ph:
                wos = WS(ph, "wo", [128, 8, 128])
                aob = tile(ph, "aob", [128, 8, L], BF16)
                aobB = [Buf() for _ in range(4)]
                aov = ao_d.rearrange("(c two) d t -> two d c t", two=2)
                for tb in range(4):
                    t0, n = TBS[tb]
                    for two in range(2):
                        S.dma("sp", aob[two * 64:(two + 1) * 64, :, t0:t0 + n], aov[two][:, :, t0:t0 + n], writes=[aobB[tb]])
                it = 0
                wov = wo_d.rearrange("h d n -> (h d) n")
                for m in range(8):
                    wt, wB = wos.load(wov[:, m * 128:(m + 1) * 128].rearrange("(c p) n -> p c n", p=128))
                    for tb in range(4):
                        t0, n = TBS[tb]
                        pp, ppB = PS[it % 4], PB[it % 4]
                        it += 1
                        S.op("pe", MMS([(pp[:, :], wt[:, c, :], aob[:, c, t0:t0 + n], c == 0, c == 7) for c in range(8)]),
                             reads=[wB, aobB[tb]], writes=[ppB])
                        S.op("dve", STT(XH["t"][:, m, t0:t0 + n], pp[:, :], mod[:, 1, 16 + m, 0:1], XH["t"][:, m, t0:t0 + n], ALU.mult, ALU.add),
                             reads=[ppB, modB, xB[m][tb]], writes=[xB[m][tb]])
                S.emit()
        if stop_after == 5:
            dump()

        if stop_after >= 6:
            with contextlib.ExitStack() as hsc:
                hT = tile(hsc, "hT", [128, 8, L], BF16)
                hB = [Buf() for _ in range(4)]
                gT = tile(hsc, "gT", [8, L], F32)
                gTB = Buf()
                selt = tile(hsc, "selt", [8, 8, 128], F32)
                selB = Buf()
                S.dma("sp", selt[:], sel_d, writes=[selB])
                with contextlib.ExitStack() as ph:
                    rt = tile(ph, "rt", [128, 8, NE], F32)
                    rtB = Buf()
                    S.dma("sp", rt[:], rout_d.rearrange("(k p) e -> p k e", p=128), writes=[rtB])
                    h32 = [tile(ph, "h32", [128, 8, 512], F32) for _ in range(2)]
                    h32B = [Buf(), Buf()]
                    lg = tile(ph, "lg", [128, 16, 8], F32)
                    mx = tile(ph, "mx", [128, 16, 8], F32)
                    gs_ = tile(ph, "gs", [128, 16, 8], F32)
                    sm = tile(ph, "sm", [128, 16, 4], F32)
                    gB = [Buf() for _ in range(16)]

                    def router(i, tb, t, tB):
                        for c4 in range(4):
                            tc = tb * 4 + c4
                            pp, ppB = PS[2 + tc % 2], PB[2 + tc % 2]
                            S.op("pe", MMS([(pp[:, 0:8], t[:, k, c4 * 128:(c4 + 1) * 128], rt[:, k, :], k == 0, k == 7) for k in range(8)]),
                                 reads=[tB, rtB], writes=[ppB])
                            S.op("dve", CP(lg[:, tc, :], pp[:, 0:8]), reads=[ppB], writes=[gB[tc]])
                            S.op("dve", lambda e, tc=tc: e.max(out=mx[:, tc, :], in_=lg[:, tc, :]), reads=[gB[tc]], writes=[gB[tc]])
                            S.op("dve", TS(sm[:, tc, 0:1], mx[:, tc, 0:1], -1.0, None, ALU.mult), reads=[gB[tc]], writes=[gB[tc]])
                            S.op("act", ACT(gs_[:, tc, :], lg[:, tc, :], AF.Exp, bias=sm[:, tc, 0:1]), reads=[gB[tc]], writes=[gB[tc]])
                            S.op("dve", STT(gs_[:, tc, :], lg[:, tc, :], mx[:, tc, 1:2], gs_[:, tc, :], ALU.is_ge, ALU.mult), reads=[gB[tc]], writes=[gB[tc]])
                            S.op("dve", lambda e, tc=tc: e.reduce_sum(out=sm[:, tc, 1:2], in_=gs_[:, tc, :], axis=mybir.AxisListType.X), reads=[gB[tc]], writes=[gB[tc]])
                            S.op("dve", lambda e, tc=tc: e.reciprocal(out=sm[:, tc, 2:3], in_=sm[:, tc, 1:2]), reads=[gB[tc]], writes=[gB[tc]])
                            S.op("dve", TS(gs_[:, tc, :], gs_[:, tc, :], sm[:, tc, 2:3], None, ALU.mult), reads=[gB[tc]], writes=[gB[tc]])
                            S.op("pe", TR(PS[4 + tc % 2][0:8, 0:128], gs_[:, tc, :], ident_f[:]), reads=[gB[tc], cB], writes=[PB[4 + tc % 2]])
                            S.op("act", ACT(gT[:, tc * 128:(tc + 1) * 128], PS[4 + tc % 2][0:8, 0:128], AF.Copy), reads=[PB[4 + tc % 2]], writes=[gTB])

                    norm_mod(ph, range(4), 1, 1, 24, hT, hB, h32=h32, h32B=h32B, after=router, nb=2)
                    S.emit()
                with contextlib.ExitStack() as ph:
                    gbc = [tile(ph, "gbc", [128, L], BF16) for _ in range(2)]
                    gbcB = [Buf(), Buf()]
                    tmp = [tile(ph, "mtmp", [128, 512], F32) for _ in range(2)]
                    tmpB = [Buf(), Buf()]
                    ti = [0]
                    FR = ffn_alloc(ph, L)
                    for e_ in range(NE):
                        g_, gB_ = gbc[e_ % 2], gbcB[e_ % 2]
                        for tb in range(4):
                            t0, n = TBS[tb]
                            S.op("pe", MMS([(PS[6][:, :], selt[:, e_, :], gT[:, t0:t0 + n], True, True)]), reads=[selB, gTB], writes=[PB[6]])
                            S.op("act", CPACT(g_[:, t0:t0 + n], PS[6][:, :]), reads=[PB[6]], writes=[gB_])

                        def gate1(m, tb, pp, ppB, t0, n, g_=g_, gB_=gB_):
                            tt, ttB = tmp[ti[0] % 2], tmpB[ti[0] % 2]
                            ti[0] += 1
                            S.op("dve", STT(tt[:, :n], pp[:, :n], mod[:, 1, 40 + m, 0:1], g_[:, t0:t0 + n], ALU.mult, ALU.mult),
                                 reads=[ppB, modB, gB_], writes=[ttB])
                            S.op("dve", TT(XH["t"][:, m, t0:t0 + n], XH["t"][:, m, t0:t0 + n], tt[:, :n], ALU.add), reads=[ttB, xB[m][tb]], writes=[xB[m][tb]])
                        ffn(FR, hT, hB, range(4), mw1_d[e_], mw3_d[e_], mw2_d[e_], gate1)
                    S.emit()
        if stop_after == 6:
            dump()

        if stop_after >= 7:
            with contextlib.ExitStack() as ph:
                sq = [tile(ph, "fsq", [128, 8, 512], BF16) for _ in range(2)]
                sqB = [Buf(), Buf()]
                rs = [tile(ph, "frs", [128, 512], F32) for _ in range(2)]
                rsB = [Buf(), Buf()]
                ot = [tile(ph, "fot", [128, 8, 512], F32) for _ in range(2)]
                otB = [Buf(), Buf()]
                for tb in range(4):
                    t0, n = TBS[tb]
                    i2 = tb % 2
                    S.op("act", ACT(sq[i2][:], XH["t"][:, :, t0:t0 + n], AF.Square), reads=[xB[k][tb] for k in range(8)], writes=[sqB[i2]])
                    S.op("pe", MMS([(PS[i2][:, :], ones_b[:], sq[i2][:, k, :], k == 0, k == 7) for k in range(8)]), reads=[sqB[i2], cB], writes=[PB[i2]])
                    S.op("act", ACT(rs[i2][:], PS[i2][:, :], AF.Ln, scale=1.0 / D, bias=V("eps")), reads=[PB[i2], vecB], writes=[rsB[i2]])
                    S.op("act", ACT(rs[i2][:], rs[i2][:], AF.Exp, scale=-0.5), reads=[rsB[i2]], writes=[rsB[i2]])
                    for k in range(8):
                        S.op("dve", STT(ot[i2][:, k, :], XH["t"][:, k, t0:t0 + n], V("nfin", k), rs[i2][:], ALU.mult, ALU.mult),
                             reads=[xB[k][tb], vecB, rsB[i2]], writes=[otB[i2]])
                    for k in range(8):
                        S.dma("sp", yT_d[k * 128:(k + 1) * 128, t0:t0 + n], ot[i2][:, k, :], reads=[otB[i2]])
                S.emit()
    return nc


def CPACT(out, in_):
    return lambda e: e.activation(out=out, in_=in_, func=AF.Copy)


def _bf(a):
    return np.ascontiguousarray(a.astype(ml_dtypes.bfloat16))


def _dft_tables(Lf):
    NT = Lf // 128
    N2 = 2 * Lf
    s = np.arange(Lf, dtype=np.float64)
    f = np.arange(Lf, dtype=np.float64) + 0.5
    ang = 2 * np.pi * np.outer(s, f) / N2
    C, Sn = np.cos(ang), np.sin(ang)
    def fwd(M):
        return M.reshape(NT, 128, NT, 128).transpose(2, 1, 0, 3)
    F = np.stack([fwd(C), fwd(Sn)], axis=2)
    def inv(M):
        return M.T.reshape(NT, 128, Lf)
    I = np.stack([inv(C), inv(Sn)], axis=2)
    return _bf(F), _bf(I)


def _zpos(Lf):
    pos = np.arange(Lf, dtype=np.float32)
    t = (pos / max(Lf - 1, 1))[:, None]
    w = (2.0 * np.pi * pos / Lf).astype(np.float32)
    f = np.linspace(1e-4, 7, 8, dtype=np.float32)
    ang = w[:, None] * f[None, :]
    z = np.concatenate([t, np.cos(ang), -np.sin(ang)], axis=-1).astype(np.float32)
    return np.ascontiguousarray(z.T)


def _cols(v):
    v = np.asarray(v, np.float32).reshape(-1)
    n = v.size
    if n < 128:
        o = np.zeros((128, 1), np.float32)
        o[:n, 0] = v
        return o
    return np.ascontiguousarray(v.reshape(n // 128, 128).T)


_CONST = {}


def _constants():
    if _CONST:
        return _CONST
    Fx, Ix = _dft_tables(L)
    Fc, Ic = _dft_tables(LC)
    deltas = np.abs(np.linspace(np.log(1e-2) / 1.5, np.log(1e-2) / 0.3, D, dtype=np.float32))
    rows = L // 64
    row = np.broadcast_to(np.arange(rows, dtype=np.float32)[:, None], (rows, 64)).reshape(L)
    col = np.broadcast_to(np.arange(64, dtype=np.float32)[None, :], (rows, 64)).reshape(L)
    inv = (10000.0 ** (-np.arange(0, 16, 2, dtype=np.float32) / 16)).astype(np.float32)
    ang = np.concatenate([row[:, None] * inv, col[:, None] * inv], axis=-1)
    cosT, sinT = np.cos(ang).T, np.sin(ang).T
    rc = np.repeat(cosT, 2, axis=0)
    rsn = np.repeat(sinT, 2, axis=0)
    rsn[0::2] *= -1.0
    sel = np.zeros((8, 8, 128), np.float32)
    for e in range(8):
        sel[e, e, :] = 1.0
    _CONST.update(dict(
        dftf_x=Fx, dfti_x=Ix, dftf_c=Fc, dfti_c=Ic,
        zpos_x=_zpos(L), zpos_c=_zpos(LC),
        delta_bc=np.ascontiguousarray(np.broadcast_to(deltas[None, :], (128, D))).astype(np.float32),
        ropeC=_bf(rc), ropeS=_bf(rsn), sel=sel,
        ropeCS=_bf(np.concatenate([np.ones((64, L), np.float32), rc, rsn], axis=0)),
        shiftm=_bf(np.concatenate([np.eye(96, dtype=np.float32), np.eye(96, dtype=np.float32)[64:96]], axis=0)),
        tnx=_cols(-np.arange(L, dtype=np.float32) / (L - 1)), tnc=_cols(-np.arange(LC, dtype=np.float32) / (LC - 1)),
    ))
    return _CONST


def _swap_pairs(a):
    sh = a.shape
    return np.ascontiguousarray(a.reshape(sh[:-1] + (sh[-1] // 2, 2))[..., ::-1].reshape(sh))


def make_in_maps(inp, ncores=8, need_moe=True):
    C = _constants()
    f32 = lambda a: np.ascontiguousarray(np.asarray(a, np.float32))
    wq_b = f32(inp["mla_wq_b"][0]).reshape(384, 16, 96)
    wq_b_sw = wq_b.copy()
    wq_b_sw[:, :, 64:] = _swap_pairs(wq_b[:, :, 64:])
    wkv_a = f32(inp["mla_wkv_a"][0])
    wkpe = np.concatenate([wkv_a[:, 0:64], wkv_a[:, 256:288]], axis=1)
    wkpe_sw = np.concatenate([wkv_a[:, 0:64], _swap_pairs(wkv_a[:, 256:288])], axis=1)
    wkv_b = f32(inp["mla_wkv_b"][0]).reshape(256, 16, 128)
    shared = dict(
        fbias_row=f32(inp["hy_f_bias"][0]).reshape(1, D), delta_bc=C["delta_bc"],
        zpos_x=C["zpos_x"], zpos_c=C["zpos_c"], dftf_x=C["dftf_x"], dftf_c=C["dftf_c"],
        dfti_x=C["dfti_x"], dfti_c=C["dfti_c"], ropeC=C["ropeC"], ropeS=C["ropeS"], sel=C["sel"],
        ropeCS=C["ropeCS"], shiftm=C["shiftm"], wq_b2=np.ascontiguousarray(np.concatenate([wq_b, wq_b_sw[:, :, 64:]], axis=2)),
        ada_w=f32(inp["ada_w"]), hy_in_w=f32(inp["hy_in_w"][0]), hy_f_w0=f32(inp["hy_f_w0"][0]),
        hy_f_wi=f32(inp["hy_f_wi"][0]), hy_f_wout=f32(inp["hy_f_wout"][0]), hy_out_w=f32(inp["hy_out_w"][0]),
        ffn_w1=f32(inp["ffn_w1"][0]), ffn_w3=f32(inp["ffn_w3"][0]), ffn_w2=f32(inp["ffn_w2"][0]),
        wq_a=f32(inp["mla_wq_a"][0]),
        wkv_a=np.ascontiguousarray(wkv_a[:, 0:256]), wkpe=np.ascontiguousarray(wkpe), wkpe_sw=np.ascontiguousarray(wkpe_sw),
        wkb_nope=np.ascontiguousarray(wkv_b[:, :, 0:64]), wvb=np.ascontiguousarray(wkv_b[:, :, 64:128].reshape(256, 1024)),
        wo=f32(inp["mla_wo"][0]).reshape(16, 64, D), router=f32(inp["moe_router"][0]),
        moe_w1=f32(inp["moe_w1"][0]), moe_w3=f32(inp["moe_w3"][0]), moe_w2=f32(inp["moe_w2"][0]),
    )
    maps = []
    for b in range(ncores):
        vec = np.zeros((128, NV), np.float32)

        def put(name, arr):
            o, c = VOFF[name]
            assert arr.shape == (128, c), (name, arr.shape, c)
            vec[:, o:o + c] = arr
        cc = _cols(inp["c"][b])
        cx = _cols(inp["c_ctx"])
        c2 = np.zeros((128, 16), np.float32)
        c2[:, 0::2] = cc
        c2[:, 1::2] = cx
        put("c2", c2)
        put("adab0", _cols(inp["ada_b"][0]))
        put("adab1", _cols(inp["ada_b"][1]))
        for l in range(2):
            put(f"nmix{l}", _cols(inp["norm_mix"][l]))
            put(f"nffn{l}", _cols(inp["norm_ffn"][l]))
        put("inb", _cols(inp["hy_in_b"][0]))
        for j in range(3):
            put(f"scw{j}", _cols(inp["hy_sc_w"][0][j]))
        put("scb", _cols(inp["hy_sc_b"][0]))
        put("outb", _cols(inp["hy_out_b"][0]))
        put("qnorm", _cols(inp["mla_q_norm"][0]))
        put("kvnorm", _cols(inp["mla_kv_norm"][0]))
        put("nfin", _cols(inp["norm_final"]))
        put("fb0", _cols(inp["hy_f_b0"][0]))
        put("fbi0", _cols(inp["hy_f_bi"][0][0]))
        put("fbi1", _cols(inp["hy_f_bi"][0][1]))
        put("ffreq", _cols(inp["hy_f_freq"][0]))
        put("eps", np.full((128, 1), 1e-6, np.float32))
        put("tnx", C["tnx"])
        put("tnc", C["tnc"])
        m = dict(shared)
        m["xT"] = np.ascontiguousarray(np.asarray(inp["x"][b], np.float32).T)
        m["cT"] = np.ascontiguousarray(np.asarray(inp["ctx"][b], np.float32).T)
        m["vec"] = vec
        maps.append(m)
    return maps


_NC = {}


def kernel(**inputs):
    if "nc" not in _NC:
        _NC["nc"] = build()
    maps = make_in_maps(inputs)
    res = run_bass_kernel_spmd(_NC["nc"], maps, core_ids=list(range(8)))
    out = np.stack([np.ascontiguousarray(res.results[b]["yT"].T) for b in range(8)], axis=0)
    return out.astype(np.float32)
```

```python
import contextlib
import numpy as np
import ml_dtypes
import concourse.bass as bass
import concourse.mybir as mybir
from concourse.bass_utils import run_bass_kernel_spmd

F32 = mybir.dt.float32
BF16 = mybir.dt.bfloat16
I32 = mybir.dt.int32
AF = mybir.ActivationFunctionType
ALU = mybir.AluOpType

D = 1024
L = 2048
LC = 256
NTOK = L + LC
DFF = 3584
NE = 8
TBS = [(0, 512), (512, 512), (1024, 512), (1536, 512), (2048, 256)]
TWO_PI = float(2 * np.pi)


class Buf:
    __slots__ = ("w", "r")

    def __init__(self):
        self.w = None
        self.r = {}


class Sched:
    ENG = ("pe", "act", "dve", "pool", "sp")
    NDQ = 8

    def __init__(self, nc, stack):
        self.nc = nc
        self.prog = {e: [] for e in self.ENG}
        self.cnt = {e: 0 for e in self.ENG}
        self.sem = {e: stack.enter_context(nc.semaphore(f"s_{e}")) for e in ("pe", "act", "dve", "pool")}
        self.dq = {}
        for q in ("sp", "pool"):
            self.dq[q] = dict(n=0, sems=[stack.enter_context(nc.semaphore(f"d_{q}{i}")) for i in range(self.NDQ)])
        self.seen = {e: {} for e in self.ENG}

    def _semobj(self, key):
        return self.sem[key[1]] if key[0] == "e" else self.dq[key[1]]["sems"][key[2]]

    def _collect(self, eng, reads, writes):
        waits = {}

        def need(key, val):
            if key == ("e", "pe") and eng == "pe":
                return
            if self.seen[eng].get(key, 0) >= val:
                return
            if waits.get(key, 0) < val:
                waits[key] = val

        for b in reads:
            if b.w is not None:
                need(*b.w)
        for b in writes:
            if b.w is not None:
                need(*b.w)
            for k, v in b.r.items():
                if k == ("e", eng):
                    continue
                need(k, v)
        for key, val in waits.items():
            self.seen[eng][key] = val
        return [(self._semobj(k), v) for k, v in waits.items()]

    def op(self, eng, fn, reads=(), writes=()):
        waits = self._collect(eng, reads, writes)
        self.cnt[eng] += 1
        key, val = ("e", eng), self.cnt[eng]
        for b in reads:
            if b.r.get(key, 0) < val:
                b.r[key] = val
        for b in writes:
            b.w = (key, val)
            b.r = {}
        self.prog[eng].append((waits, fn, self.sem[eng], 1))

    def dma(self, q, out, in_, reads=(), writes=()):
        d = self.dq[q]
        n = d["n"]
        d["n"] += 1
        slot = n % self.NDQ
        val = 16 * (n // self.NDQ + 1)
        waits = self._collect(q, reads, writes)
        key = ("d", q, slot)
        if n >= self.NDQ and self.seen[q].get(key, 0) < val - 16:
            waits.append((d["sems"][slot], val - 16))
            self.seen[q][key] = val - 16
        for b in reads:
            b.r[key] = val
        for b in writes:
            b.w = (key, val)
            b.r = {}
        self.prog[q].append((waits, (lambda e: e.dma_start(out=out, in_=in_)), d["sems"][slot], 16))

    def flush_dma(self):
        for q, d in self.dq.items():
            n = d["n"]
            waits = []
            for slot in range(self.NDQ):
                cs = (n - slot + self.NDQ - 1) // self.NDQ if n > slot else 0
                if cs > 0:
                    key = ("d", q, slot)
                    val = 16 * cs
                    if self.seen[q].get(key, 0) < val:
                        waits.append((d["sems"][slot], val))
                        self.seen[q][key] = val
            if waits:
                self.prog[q].append((waits, None, None, 0))

    def emit(self):
        self.flush_dma()
        prog = self.prog
        self.prog = {e: [] for e in self.ENG}
        with self.nc.Block() as block:
            def mk(e):
                def body(eng):
                    for waits, fn, sem, amt in prog[e]:
                        for s, v in waits:
                            eng.wait_ge(s, v)
                        if fn is not None:
                            fn(eng).then_inc(sem, amt)
                return body
            block.tensor(mk("pe"))
            block.scalar(mk("act"))
            block.vector(mk("dve"))
            block.gpsimd(mk("pool"))
            block.sync(mk("sp"))


def MMS(mms):
    def fn(e):
        ins = None
        for (o, l, r, st, sp) in mms:
            ins = e.matmul(o, l, r, start=st, stop=sp)
        return ins
    return fn


def TR(out, in_, ident):
    return lambda e: e.transpose(out, in_, ident)


def ACT(out, in_, func, scale=None, bias=None):
    kw = {}
    if scale is not None:
        kw["scale"] = scale
    if bias is not None:
        kw["bias"] = bias
    return lambda e: e.activation(out=out, in_=in_, func=func, **kw)


def TS(out, in0, s1, s2, op0, op1=None):
    if op1 is None:
        return lambda e: e.tensor_scalar(out=out, in0=in0, scalar1=s1, scalar2=None, op0=op0)
    return lambda e: e.tensor_scalar(out=out, in0=in0, scalar1=s1, scalar2=s2, op0=op0, op1=op1)


def TT(out, in0, in1, op):
    return lambda e: e.tensor_tensor(out=out, in0=in0, in1=in1, op=op)


def STT(out, in0, scalar, in1, op0, op1):
    return lambda e: e.scalar_tensor_tensor(out=out, in0=in0, scalar=scalar, in1=in1, op0=op0, op1=op1)


def CP(out, in_):
    return lambda e: e.tensor_copy(out=out, in_=in_)


def MSET(ap, v):
    return lambda e: e.memset(ap, v)


VEC_SPEC = [("c2", 16), ("adab0", 48), ("adab1", 48), ("nmix0", 8), ("nmix1", 8), ("nffn0", 8), ("nffn1", 8),
            ("inb", 24), ("scw0", 24), ("scw1", 24), ("scw2", 24), ("scb", 24), ("outb", 8), ("qnorm", 3),
            ("kvnorm", 2), ("nfin", 8), ("fb0", 1), ("fbi0", 1), ("fbi1", 1), ("ffreq", 1), ("eps", 1),
            ("tnx", 16), ("tnc", 2)]
VOFF = {}
_o = 0
for _n, _c in VEC_SPEC:
    VOFF[_n] = (_o, _c)
    _o += _c
NV = _o


def build(stop_after=99, dbg=False):
    nc = bass.Bass("TRN2", target_bir_lowering=False)

    def din(name, shape, dt=F32):
        return nc.dram_tensor(name, list(shape), dt, kind="ExternalInput").ap()

    def dscr(name, shape, dt):
        return nc.dram_tensor(name, list(shape), dt, kind="Internal").ap()

    xT_d = din("xT", [D, L])
    cT_d = din("cT", [D, LC])
    vec_d = din("vec", [128, NV])
    fbr_d = din("fbias_row", [1, D])
    dbc_d = din("delta_bc", [128, D])
    zpos_d = {L: din("zpos_x", [17, L]), LC: din("zpos_c", [17, LC])}
    dftf_d = {L: din("dftf_x", [16, 128, 2, 16, 128], BF16), LC: din("dftf_c", [2, 128, 2, 2, 128], BF16)}
    dfti_d = {L: din("dfti_x", [16, 128, 2, L], BF16), LC: din("dfti_c", [2, 128, 2, LC], BF16)}
    ropeC_d = din("ropeC", [32, L], BF16)
    ropeS_d = din("ropeS", [32, L], BF16)
    sel_d = din("sel", [8, 8, 128])
    ada_w_d = din("ada_w", [2, D, 6 * D])
    in_w_d = din("hy_in_w", [D, 3 * D])
    fw0_d = din("hy_f_w0", [17, 64])
    fwi_d = din("hy_f_wi", [2, 64, 64])
    fwout_d = din("hy_f_wout", [64, 2 * D])
    out_w_d = din("hy_out_w", [D, D])
    w1_d = din("ffn_w1", [D, DFF])
    w3_d = din("ffn_w3", [D, DFF])
    w2_d = din("ffn_w2", [DFF, D])
    wqa_d = din("wq_a", [D, 384])
    wqb2_d = din("wq_b2", [384, 16, 128])
    ropeCS_d = din("ropeCS", [128, L], BF16)
    shift_d = din("shiftm", [128, 96], BF16)
    wkva_d = din("wkv_a", [D, 256])
    wkpe_d = din("wkpe", [D, 96])
    wkpes_d = din("wkpe_sw", [D, 96])
    wkbn_d = din("wkb_nope", [256, 16, 64])
    wvb_d = din("wvb", [256, 1024])
    wo_d = din("wo", [16, 64, D])
    rout_d = din("router", [D, NE])
    mw1_d = din("moe_w1", [NE, D, DFF]) if stop_after >= 6 else None
    mw3_d = din("moe_w3", [NE, D, DFF]) if stop_after >= 6 else None
    mw2_d = din("moe_w2", [NE, DFF, D]) if stop_after >= 6 else None
    yT_d = nc.dram_tensor("yT", [D, L], F32, kind="ExternalOutput").ap()
    dbg_d = nc.dram_tensor("dbg", [D, NTOK], F32, kind="ExternalOutput").ap() if dbg else None

    pq_d = {L: dscr("pq_x", [16, 128, 2, D], BF16), LC: dscr("pq_c", [2, 128, 2, D], BF16)}
    x0_d = dscr("x0_s", [8, 128, NTOK], BF16)
    u_d = dscr("u_s", [8, 128, NTOK], BF16)
    q_d = dscr("q_s", [16, 96, L], BF16)
    k_d = dscr("k_s", [16, 96, NTOK], BF16)
    ao_d = dscr("ao_s", [16, 64, L], BF16)

    with contextlib.ExitStack() as gs:
        S = Sched(nc, gs)
        cnt = [0]

        def tile(st, name, shape, dt):
            cnt[0] += 1
            return st.enter_context(nc.sbuf_tensor(f"{name}_{cnt[0]}", list(shape), dt))

        PS = [gs.enter_context(nc.psum_tensor(f"ps{i}", [128, 512], F32)) for i in range(7)]
        PSB = gs.enter_context(nc.psum_tensor("psb", [128, 1024], BF16))
        PB = [Buf() for _ in range(7)]
        PBB = [Buf(), Buf()]
        PS6b = PS[6].bitcast(BF16)

        xs_d = dscr("x_spill", [8, 128, NTOK], F32)
        XH = {"t": None}
        xB = [[Buf() for _ in TBS] for _ in range(8)]
        vec = tile(gs, "vec", [128, NV], F32)
        vecB = Buf()
        mod = tile(gs, "mod", [128, 2, 48, 2], F32)
        modB = Buf()
        der = tile(gs, "der", [128, 2, 2, 3, 8], F32)
        derB = Buf()
        invn = tile(gs, "invn", [128, 2, 8], F32)
        invnB = Buf()
        ones_f = tile(gs, "ones_f", [128, 128], F32)
        ones_b = tile(gs, "ones_b", [128, 128], BF16)
        ident_f = tile(gs, "ident_f", [128, 128], F32)
        ident_b = tile(gs, "ident_b", [128, 128], BF16)
        cB = Buf()

        def V(name, j=0, n=1):
            o, c = VOFF[name]
            return vec[:, o + j:o + j + n]

        def V64(name):
            o, c = VOFF[name]
            return vec[0:64, o:o + 1]

        class WS:
            def __init__(self, st, name, shape, nst=2, nbf=2, cast=True, ceng="act"):
                self.cast = cast
                self.ceng = ceng
                self.st = [tile(st, name + "s", shape, F32) for _ in range(nst)]
                self.sB = [Buf() for _ in range(nst)]
                if cast:
                    self.bf = [tile(st, name + "b", shape, BF16) for _ in range(nbf)]
                    self.bB = [Buf() for _ in range(nbf)]
                self.i = 0

            def issue(self, src):
                i = self.i
                self.i += 1
                s, sb = self.st[i % len(self.st)], self.sB[i % len(self.st)]
                S.dma("sp", s[:], src, writes=[sb])
                return i

            def finish(self, i):
                s, sb = self.st[i % len(self.st)], self.sB[i % len(self.st)]
                b, bb = self.bf[i % len(self.bf)], self.bB[i % len(self.bf)]
                S.op("act", CPACT(b[:], s[:]), reads=[sb], writes=[bb])
                return b, bb

            def load(self, src, sl=None):
                i = self.i
                self.i += 1
                s, sb = self.st[i % len(self.st)], self.sB[i % len(self.st)]
                sv = s[:] if sl is None else sl(s)
                S.dma("sp", sv, src, writes=[sb])
                if not self.cast:
                    return s, sb
                b, bb = self.bf[i % len(self.bf)], self.bB[i % len(self.bf)]
                bv = b[:] if sl is None else sl(b)
                if self.ceng == "act":
                    S.op("act", CPACT(bv, sv), reads=[sb], writes=[bb])
                else:
                    S.op(self.ceng, CP(bv, sv), reads=[sb], writes=[bb])
                return b, bb

        def dump(k_list=range(8)):
            if dbg:
                for k in k_list:
                    S.dma("sp", dbg_d[k * 128:(k + 1) * 128, :], XH["t"][:, k, :], reads=xB[k])
                S.emit()

        with contextlib.ExitStack() as ph:
            S.dma("sp", vec[:], vec_d, writes=[vecB])
            S.op("pool", MSET(ones_f[:], 1.0), writes=[cB])
            S.op("pool", CP(ones_b[:], ones_f[:]), reads=[cB], writes=[cB])
            S.op("pool", MSET(ident_f[:], 1.0), reads=[cB], writes=[cB])
            S.op("pool", lambda e: e.affine_select(out=ident_f[:], in_=ident_f[:], pattern=[[-1, 128]],
                                                    compare_op=ALU.is_equal, fill=0.0, base=0, channel_multiplier=1),
                 reads=[cB], writes=[cB])
            S.op("pool", CP(ident_b[:], ident_f[:]), reads=[cB], writes=[cB])
            c2 = tile(gs, "c2", [128, 16], F32)
            c2B = Buf()
            S.op("act", ACT(c2[:], V("c2", 0, 16), AF.Silu), reads=[vecB], writes=[c2B])
            S.emit()

        PSBf = PSB.bitcast(F32)

        c2b = tile(gs, "c2b", [128, 16], BF16)
        S.op("act", CPACT(c2b[:], c2[:]), reads=[c2B], writes=[c2B])

        def ada_block(adaW, bi):
            l, cb = bi // 24, bi % 24
            wt, wb = adaW.load(ada_w_d[l][:, cb * 256:(cb + 1) * 256].rearrange("(k p) n -> p k n", p=128))
            mms = []
            for j in range(2):
                for k in range(8):
                    mms.append((PSBf[:, 2 * j:2 * j + 2], wt[:, k, j * 128:(j + 1) * 128], c2b[:, 2 * k:2 * k + 2], k == 0, k == 7))
            S.op("pe", MMS(mms), reads=[wb, c2B], writes=[PBB[0]])
            for j in range(2):
                jj = cb * 2 + j
                S.op("dve", TS(mod[:, l, jj, :], PSBf[:, 2 * j:2 * j + 2], V(f"adab{l}", jj), None, ALU.add),
                     reads=[PBB[0], vecB], writes=[modB])

        def ada_derived():
            for l in range(2):
                for s in range(2):
                    S.op("dve", STT(der[:, l, s, 0, :], mod[:, l, 8:16, s], 1.0, V(f"nmix{l}", 0, 8), ALU.add, ALU.mult),
                         reads=[modB, vecB], writes=[derB])
                    S.op("dve", STT(der[:, l, s, 1, :], mod[:, l, 32:40, s], 1.0, V(f"nffn{l}", 0, 8), ALU.add, ALU.mult),
                         reads=[modB, vecB], writes=[derB])
            for s in range(2):
                S.op("dve", TT(der[:, 0, s, 2, :], mod[:, 0, 16:24, s], V("outb", 0, 8), ALU.mult),
                     reads=[modB, vecB], writes=[derB])
            S.emit()

        def norm_mod(ph, tbs, l, kind, shift_j0, hT, hB, h32=None, h32B=None, after=None, nb=1):
            sq = [tile(ph, "sq", [128, 8, 512], BF16) for _ in range(nb)] * (3 - nb)
            sqB = [Buf() for _ in range(nb)] * (3 - nb)
            rs = [tile(ph, "rs", [128, 512], F32) for _ in range(2)]
            rsB = [Buf(), Buf()]
            t32 = [tile(ph, "t32", [128, 8, 512], F32) for _ in range(nb)] * (3 - nb) if h32 is None else h32
            t32B = [Buf() for _ in range(nb)] * (3 - nb) if h32 is None else h32B
            for i, tb in enumerate(tbs):
                t0, n = TBS[tb]
                s = 0 if tb < 4 else 1
                q, qB = sq[i % 2], sqB[i % 2]
                r, rB = rs[i % 2], rsB[i % 2]
                t, tB = t32[i % 2], t32B[i % 2]
                pp, ppB = PS[i % 2], PB[i % 2]
                xr = [xB[k][tb] for k in range(8)]
                S.op("act", ACT(q[:, :, :n], XH["t"][:, :, t0:t0 + n], AF.Square), reads=xr, writes=[qB])
                S.op("pe", MMS([(pp[:, :n], ones_b[:], q[:, k, :n], k == 0, k == 7) for k in range(8)]),
                     reads=[qB, cB], writes=[ppB])
                S.op("act", ACT(r[:, :n], pp[:, :n], AF.Ln, scale=1.0 / D, bias=V("eps")), reads=[ppB, vecB], writes=[rB])
                S.op("act", ACT(r[:, :n], r[:, :n], AF.Exp, scale=-0.5), reads=[rB], writes=[rB])
                for k in range(8):
                    S.op("dve", TT(t[:, k, :n], XH["t"][:, k, t0:t0 + n], r[:, :n], ALU.mult), reads=[xB[k][tb], rB], writes=[tB])
                for k in range(8):
                    if h32 is None:
                        S.op("act", ACT(hT[:, k, t0:t0 + n], t[:, k, :n], AF.Identity, scale=der[:, l, s, kind, k:k + 1],
                                        bias=mod[:, l, shift_j0 + k, s:s + 1]), reads=[tB, derB, modB], writes=[hB[tb]])
                    else:
                        S.op("act", ACT(t[:, k, :n], t[:, k, :n], AF.Identity, scale=der[:, l, s, kind, k:k + 1],
                                        bias=mod[:, l, shift_j0 + k, s:s + 1]), reads=[tB, derB, modB], writes=[tB])
                if h32 is not None:
                    S.op("pool", CP(hT[:, :, t0:t0 + n], t[:, :, :n]), reads=[tB], writes=[hB[tb]])
                    after(i, tb, t, tB)

        def filter_phase(Lf, which):
            NT = Lf // 128
            N2 = 2 * Lf
            with contextlib.ExitStack() as ph:
                zp = tile(ph, "zp", [17, Lf], F32)
                w0 = tile(ph, "w0", [17, 64], F32)
                wi = tile(ph, "wi", [64, 2, 64], F32)
                wout = tile(ph, "wout", [64, 2 * D], F32)
                dbc = tile(ph, "dbc", [128, D], F32)
                fbr = tile(ph, "fbr", [1, D], F32)
                ldB = Buf()
                S.dma("sp", zp[:], zpos_d[Lf], writes=[ldB])
                S.dma("sp", w0[:], fw0_d, writes=[ldB])
                S.dma("sp", wi[:], fwi_d.rearrange("n k m -> k n m"), writes=[ldB])
                S.dma("sp", wout[:], fwout_d, writes=[ldB])
                S.dma("sp", dbc[:], dbc_d, writes=[ldB])
                S.dma("sp", fbr[:], fbr_d, writes=[ldB])
                fsv = tile(ph, "fsv", [64, 4], F32)
                fsB = Buf()
                S.op("dve", TS(fsv[:, 0:1], V64("ffreq"), 1.0 / TWO_PI, None, ALU.mult), reads=[vecB], writes=[fsB])
                for i, nm in enumerate(["fb0", "fbi0", "fbi1"]):
                    S.op("dve", TS(fsv[:, i + 1:i + 2], V64(nm), fsv[:, 0:1], 16.0, ALU.mult, ALU.add), reads=[vecB, fsB], writes=[fsB])
                hA = tile(ph, "hA", [64, Lf], F32)
                hBt = tile(ph, "hBt", [64, Lf], F32)
                hbuf = [Buf(), Buf()]
                yt = [tile(ph, "yt", [64, 512], F32) for _ in range(2)]
                qi = [tile(ph, "qi", [64, 512], I32) for _ in range(2)]
                ytB = [Buf(), Buf()]
                blocks = [(t0, min(512, Lf - t0)) for t0 in range(0, Lf, 512)]
                it = 0
                for layer in range(3):
                    lhsT = w0[:, :] if layer == 0 else wi[:, layer - 1, :]
                    src, srcB = (zp, ldB) if layer == 0 else ((hA, hbuf[0]) if layer == 1 else (hBt, hbuf[1]))
                    dst, dstB = (hA, hbuf[0]) if layer in (0, 2) else (hBt, hbuf[1])
                    for (t0, n) in blocks:
                        pp, ppB = PS[it % 2], PB[it % 2]
                        y, q, yB = yt[it % 2], qi[it % 2], ytB[it % 2]
                        it += 1
                        S.op("pe", MMS([(pp[0:64, :n], lhsT, src[:, t0:t0 + n], True, True)]), reads=[ldB, srcB], writes=[ppB])
                        S.op("dve", TS(y[:, :n], pp[0:64, :n], fsv[:, 0:1], fsv[:, layer + 1:layer + 2], ALU.mult, ALU.add),
                             reads=[ppB, fsB], writes=[yB])
                        S.op("dve", CP(q[:, :n], y[:, :n]), reads=[yB], writes=[yB])
                        S.op("dve", TT(y[:, :n], y[:, :n], q[:, :n], ALU.subtract), reads=[yB], writes=[yB])
                        S.op("act", ACT(dst[:, t0:t0 + n], y[:, :n], AF.Sin, scale=TWO_PI), reads=[yB], writes=[dstB])
                h3, h3B = hA, hbuf[0]
                ks = tile(ph, "ks", [128, NT, D], BF16)
                kd = tile(ph, "kd", [128, NT, D], BF16)
                ksB = [Buf() for _ in range(NT)]
                acc = tile(ph, "acc", [128, D], F32)
                accB = Buf()
                ks0 = tile(ph, "ks0", [1, D], F32)
                ks0B = Buf()
                S.op("pool", MSET(acc[:], 0.0), writes=[accB])
                dec = [tile(ph, "dec", [128, D], F32) for _ in range(2)]
                kfb = [tile(ph, "kfb", [128, 2 * D], F32) for _ in range(2)]
                decB = [Buf(), Buf()]
                kfbB = [Buf(), Buf()]
                kab = tile(ph, "kab", [128, 2 * D], F32)
                kabB = Buf()
                tn = "tnx" if Lf == L else "tnc"
                adaW = WS(ph, "adaw", [128, 8, 256], nst=2, nbf=2) if Lf == L else None
                for tc in range(NT):
                    if adaW is not None:
                        for b3 in range(3):
                            ada_block(adaW, 3 * tc + b3)
                    de, deB = dec[tc % 2], decB[tc % 2]
                    kk, kkB = kfb[tc % 2], kfbB[tc % 2]
                    pk = [PS[(tc % 2) * 3 + i] for i in range(3)] + [PS[6]]
                    pkB = [PB[(tc % 2) * 3 + i] for i in range(3)] + [PB[6]]
                    for qd in range(4):
                        S.op("pe", MMS([(pk[qd][:, :], h3[:, tc * 128:(tc + 1) * 128], wout[:, qd * 512:(qd + 1) * 512], True, True)]),
                             reads=[h3B, ldB], writes=[pkB[qd]])
                    S.op("act", ACT(de[:], dbc[:], AF.Exp, scale=V(tn, tc)), reads=[ldB, vecB], writes=[deB])
                    for qd in range(4):
                        S.op("dve", TT(kk[:, qd * 512:(qd + 1) * 512], pk[qd][:, :], de[:, (qd % 2) * 512:(qd % 2 + 1) * 512], ALU.mult),
                             reads=[pkB[qd], deB], writes=[kkB])
                    if tc == 0:
                        S.op("dve", MSET(kk[0:1, D:2 * D], 0.0), reads=[kkB], writes=[kkB])
                    S.op("act", ACT(kab[:], kk[:], AF.Abs), reads=[kkB], writes=[kabB])
                    S.op("dve", TT(acc[:], acc[:], kab[:, 0:D], ALU.add), reads=[kabB, accB], writes=[accB])
                    S.op("dve", TT(acc[:], acc[:], kab[:, D:2 * D], ALU.add), reads=[kabB, accB], writes=[accB])
                    S.op("pool", TT(ks[:, tc, :], kk[:, 0:D], kk[:, D:2 * D], ALU.add), reads=[kkB], writes=[ksB[tc]])
                    S.op("pool", TT(kd[:, tc, :], kk[:, D:2 * D], kk[:, 0:D], ALU.subtract), reads=[kkB], writes=[ksB[tc]])
                    if tc == 0:
                        S.op("pool", TT(ks0[:], kk[0:1, 0:D], kk[0:1, D:2 * D], ALU.add), reads=[kkB], writes=[ks0B])
                pn, pnB = PS[0], PB[0]
                S.op("pe", MMS([(pn[:, j:j + 1], acc[:, j * 128:(j + 1) * 128], ones_f[:, 0:1], True, True) for j in range(8)]),
                     reads=[accB, cB], writes=[pnB])
                S.op("dve", lambda e: e.reciprocal(out=invn[:, which, :], in_=pn[:, 0:8]), reads=[pnB], writes=[invnB])
                S.op("dve", TS(invn[:, which, :], invn[:, which, :], 2.0 / N2, None, ALU.mult), reads=[invnB], writes=[invnB])
                for hf in range(2):
                    pr, prB = PS[1 + hf], PB[1 + hf]
                    S.op("pe", MMS([(pr[0:1, :], ones_f[:, 0:1], acc[:, hf * 512:(hf + 1) * 512], True, True)]),
                         reads=[accB, cB], writes=[prB])
                    S.op("dve", TT(dec[0][0:1, hf * 512:(hf + 1) * 512], pr[0:1, :], fbr[:, hf * 512:(hf + 1) * 512], ALU.mult),
                         reads=[prB, ldB], writes=[decB[0]])
                S.op("dve", TT(ks[0:1, 0, :], dec[0][0:1, :], ks0[:], ALU.add), reads=[decB[0], ks0B], writes=[ksB[0]])
                tab = [tile(ph, "tab", [128, 2, NT, 128], BF16) for _ in range(2)]
                tabB = [Buf(), Buf()]
                pqt = [tile(ph, "pqt", [128, 2, D], BF16) for _ in range(2)]
                pqB = [Buf(), Buf()]
                S.dma("sp", tab[0][:], dftf_d[Lf][0], writes=[tabB[0]])
                for fc in range(NT):
                    tb_, tbB = tab[fc % 2], tabB[fc % 2]
                    if fc + 1 < NT:
                        S.dma("sp", tab[(fc + 1) % 2][:], dftf_d[Lf][fc + 1], writes=[tabB[(fc + 1) % 2]])
                    pq, pqb = pqt[fc % 2], pqB[fc % 2]
                    for cb in range(2):
                        for cs in range(2):
                            pi = (fc % 2) * 3 + cs if cb == 0 else ((fc % 2) * 3 + 2 if cs == 0 else 6)
                            src = ks if cs == 0 else kd
                            S.op("pe", MMS([(PS[pi][:, :], tb_[:, cs, sc, :], src[:, sc, cb * 512:(cb + 1) * 512], sc == 0, sc == NT - 1)
                                            for sc in range(NT)]), reads=[tbB] + ksB, writes=[PB[pi]])
                            eng = "act" if cs == 0 else "dve"
                            fn = ACT(pq[:, cs, cb * 512:(cb + 1) * 512], PS[pi][:, :], AF.Copy) if cs == 0 else CP(pq[:, cs, cb * 512:(cb + 1) * 512], PS[pi][:, :])
                            S.op(eng, fn, reads=[PB[pi]], writes=[pqb])
                    S.dma("sp", pq_d[Lf][fc], pq[:], reads=[pqb])
                S.emit()

        if stop_after >= 1:
            filter_phase(L, 0)
            ada_derived()
            filter_phase(LC, 1)

        if stop_after >= 2:
            with contextlib.ExitStack() as msc:
                v_tok = tile(msc, "vtok", [128, 18, D], BF16)
                vtB = [Buf() for _ in range(18)]
                with contextlib.ExitStack() as hsc:
                    hT = tile(hsc, "hT", [128, 8, NTOK], BF16)
                    hB = [Buf() for _ in TBS]
                    with contextlib.ExitStack() as ph:
                        XH["t"] = tile(ph, "xT", [128, 8, NTOK], F32)
                        for k in range(8):
                            S.dma("sp", XH["t"][:, k, 0:L], xT_d[k * 128:(k + 1) * 128, :], writes=xB[k][0:4])
                            S.dma("sp", XH["t"][:, k, L:NTOK], cT_d[k * 128:(k + 1) * 128, :], writes=[xB[k][4]])
                        norm_mod(ph, range(5), 0, 0, 0, hT, hB)
                        for k in range(8):
                            S.dma("sp", xs_d[k], XH["t"][:, k, :], reads=xB[k])
                        S.emit()
                        XH["t"] = None
                    with contextlib.ExitStack() as ph:
                        wst = WS(ph, "inw", [128, 8, 128], nst=4, nbf=4)
                        z = [tile(ph, "z", [128, NTOK + 4], F32) for _ in range(2)]
                        zB = [Buf(), Buf()]
                        for zz, zb in zip(z, zB):
                            S.op("pool", MSET(zz[:], 0.0), writes=[zb])
                        cA = tile(ph, "cA", [128, NTOK], F32)
                        cBt = tile(ph, "cBt", [128, NTOK], F32)
                        cAB, cBB = Buf(), Buf()
                        x0o = [tile(ph, "x0o", [128, NTOK], BF16) for _ in range(2)]
                        x0B = [Buf(), Buf()]
                        vTt = [tile(ph, "vTt", [128, NTOK], BF16) for _ in range(2)]
                        vTB = [Buf(), Buf()]
                        zi = 0
                        SEG = [(1, 0, L), (L + 3, L, LC)]

                        def proj_conv(m, out_ap_fn, outB):
                            nonlocal zi
                            wt, wb = wst.finish(wq_pref.pop(0))
                            if roles:
                                mm_ = roles.pop(0)
                                wq_pref.append(wst.issue(in_w_d[:, mm_ * 128:(mm_ + 1) * 128].rearrange("(k p) n -> p k n", p=128)))
                            zz, zb = z[zi % 2], zB[zi % 2]
                            zi += 1
                            for tb, (t0, n) in enumerate(TBS):
                                pp, ppB = PS[tb % 4], PB[tb % 4]
                                S.op("pe", MMS([(pp[:, :n], wt[:, k, :], hT[:, k, t0:t0 + n], k == 0, k == 7) for k in range(8)]),
                                     reads=[wb, hB[tb]], writes=[ppB])
                                zc = (1 + t0) if tb < 4 else (L + 3)
                                S.op("act", ACT(zz[:, zc:zc + n], pp[:, :n], AF.Identity, bias=V("inb", m)), reads=[ppB, vecB], writes=[zb])
                            for (zc, t0, n) in SEG:
                                o = out_ap_fn(t0, n)
                                S.op("dve", TS(tmpc[:, t0:t0 + n], zz[:, zc:zc + n], V("scw1", m), V("scb", m), ALU.mult, ALU.add),
                                     reads=[zb, vecB], writes=[tmpcB])
                                S.op("dve", STT(tmpc[:, t0:t0 + n], zz[:, zc - 1:zc - 1 + n], V("scw0", m), tmpc[:, t0:t0 + n], ALU.mult, ALU.add),
                                     reads=[zb, vecB, tmpcB], writes=[tmpcB])
                                S.op("dve", STT(o, zz[:, zc + 1:zc + 1 + n], V("scw2", m), tmpc[:, t0:t0 + n], ALU.mult, ALU.add),
                                     reads=[zb, vecB, tmpcB], writes=[outB])

                        tmpc = tile(ph, "tmpc", [128, NTOK], F32)
                        tmpcB = Buf()
                        roles = [m_ for j in range(8) for m_ in (j, 8 + j, 16 + j)]
                        wq_pref = []
                        for _ in range(3):
                            mm_ = roles.pop(0)
                            wq_pref.append(wst.issue(in_w_d[:, mm_ * 128:(mm_ + 1) * 128].rearrange("(k p) n -> p k n", p=128)))
                        for j in range(8):
                            xo, xoB = x0o[j % 2], x0B[j % 2]
                            proj_conv(j, lambda t0, n: xo[:, t0:t0 + n], xoB)
                            proj_conv(8 + j, lambda t0, n: cA[:, t0:t0 + n], cAB)
                            proj_conv(16 + j, lambda t0, n: cBt[:, t0:t0 + n], cBB)
                            S.dma("sp", x0_d[j], xo[:], reads=[xoB])
                            vt, vB = vTt[j % 2], vTB[j % 2]
                            S.op("dve", TT(vt[:], cA[:], cBt[:], ALU.mult), reads=[cAB, cBB], writes=[vB])
                            for b3 in range(3):
                                bank = PSB if b3 % 2 == 0 else PS6b
                                bB_ = PBB[b3 % 2]

                                def trs(e, bank=bank, b3=b3, vt=vt):
                                    ins = None
                                    for i6 in range(6):
                                        tc = b3 * 6 + i6
                                        ins = e.transpose(bank[:, i6 * 128:(i6 + 1) * 128], vt[:, tc * 128:(tc + 1) * 128], ident_b[:])
                                    return ins
                                S.op("pe", trs, reads=[vB, cB], writes=[bB_])
                                S.op("act", ACT(v_tok[:, b3 * 6:(b3 + 1) * 6, j * 128:(j + 1) * 128],
                                                bank[:, 0:768].rearrange("p (a b) -> p a b", a=6), AF.Copy),
                                     reads=[bB_], writes=[vtB[b3 * 6 + i6] for i6 in range(6)])
                        S.emit()

                if stop_after >= 3:
                    def longconv(Lf, which, tc0, tok0):
                        NT = Lf // 128
                        with contextlib.ExitStack() as ph:
                            Yr = tile(ph, "Yr", [128, NT, 512], BF16)
                            Yi = tile(ph, "Yi", [128, NT, 512], BF16)
                            YB = [Buf() for _ in range(NT)]
                            tab = [tile(ph, "ftab", [128, 2, NT, 128], BF16) for _ in range(2)]
                            tabB = [Buf(), Buf()]
                            pqt = [tile(ph, "pq", [128, 2, 512], BF16) for _ in range(2)]
                            pqB = [Buf(), Buf()]
                            tm = [tile(ph, "tm", [128, 4, 512], F32) for _ in range(2)]
                            tmB = [Buf(), Buf()]
                            itab = [tile(ph, "itab", [128, 2, 512], BF16) for _ in range(3)]
                            itB = [Buf() for _ in range(3)]
                            x0t = [tile(ph, "x0t", [128, 512], BF16) for _ in range(8)]
                            x0B = [Buf() for _ in range(8)]
                            ut = [tile(ph, "ut", [128, 512], BF16) for _ in range(4)]
                            utB = [Buf() for _ in range(4)]
                            blocks = [(t0, min(512, Lf - t0)) for t0 in range(0, Lf, 512)]
                            ii = 0
                            xi = 0
                            ustores = []
                            for cb in range(2):
                                for fc in range(NT):
                                    tb_, tbB = tab[fc % 2], tabB[fc % 2]
                                    S.dma("sp", tb_[:], dftf_d[Lf][fc], writes=[tbB])
                                    pq, pqb = pqt[fc % 2], pqB[fc % 2]
                                    S.dma("sp", pq[:], pq_d[Lf][fc][:, :, cb * 512:(cb + 1) * 512], writes=[pqb])
                                    pA, pAB = PS[(fc % 2) * 2], PB[(fc % 2) * 2]
                                    pBm, pBB = PS[(fc % 2) * 2 + 1], PB[(fc % 2) * 2 + 1]
                                    vr = [vtB[tc0 + sc] for sc in range(NT)]
                                    S.op("pe", MMS([(pA[:, :], tb_[:, 0, sc, :], v_tok[:, tc0 + sc, cb * 512:(cb + 1) * 512], sc == 0, sc == NT - 1)
                                                    for sc in range(NT)]), reads=[tbB] + vr, writes=[pAB])
                                    S.op("pe", MMS([(pBm[:, :], tb_[:, 1, sc, :], v_tok[:, tc0 + sc, cb * 512:(cb + 1) * 512], sc == 0, sc == NT - 1)
                                                    for sc in range(NT)]), reads=[tbB] + vr, writes=[pBB])
                                    t, tB = tm[fc % 2], tmB[fc % 2]
                                    S.op("dve", TT(t[:, 0, :], pA[:, :], pq[:, 0, :], ALU.mult), reads=[pAB, pqb], writes=[tB])
                                    S.op("dve", TT(t[:, 1, :], pBm[:, :], pq[:, 1, :], ALU.mult), reads=[pBB, pqb], writes=[tB])
                                    S.op("dve", TT(t[:, 2, :], pBm[:, :], pq[:, 0, :], ALU.mult), reads=[pBB, pqb], writes=[tB])
                                    S.op("dve", TT(t[:, 3, :], pA[:, :], pq[:, 1, :], ALU.mult), reads=[pAB, pqb], writes=[tB])
                                    S.op("pool", TT(Yr[:, fc, :], t[:, 0, :], t[:, 1, :], ALU.add), reads=[tB], writes=[YB[fc]])
                                    S.op("pool", TT(Yi[:, fc, :], t[:, 2, :], t[:, 3, :], ALU.subtract), reads=[tB], writes=[YB[fc]])
                                for (t0, n) in blocks:
                                    for j in range(4):
                                        S.dma("sp", x0t[(xi + j) % 8][:, :n], x0_d[cb * 4 + j][:, tok0 + t0:tok0 + t0 + n], writes=[x0B[(xi + j) % 8]])
                                    for fc in range(NT):
                                        itb, itb_B = itab[ii % 3], itB[ii % 3]
                                        ii += 1
                                        S.dma("sp", itb[:, :, :n], dfti_d[Lf][fc][:, :, t0:t0 + n], writes=[itb_B])
                                        if fc == min(2, NT - 1):
                                            while ustores:
                                                o_, i_, b_u = ustores.pop(0)
                                                S.dma("sp", o_, i_, reads=[b_u])
                                        for j in range(4):
                                            S.op("pe", MMS([(PS[j][:, :n], Yr[:, fc, j * 128:(j + 1) * 128], itb[:, 0, :n], fc == 0, False),
                                                            (PS[j][:, :n], Yi[:, fc, j * 128:(j + 1) * 128], itb[:, 1, :n], False, fc == NT - 1)]),
                                                 reads=[YB[fc], itb_B], writes=[PB[j]])
                                    for j in range(4):
                                        jj = cb * 4 + j
                                        xt_, xtB = x0t[xi % 8], x0B[xi % 8]
                                        uu, uB = ut[xi % 4], utB[xi % 4]
                                        xi += 1
                                        S.op("dve", STT(uu[:, :n], PS[j][:, :n], invn[:, which, jj:jj + 1], xt_[:, :n], ALU.mult, ALU.mult),
                                             reads=[PB[j], invnB, xtB], writes=[uB])
                                        ustores.append((u_d[jj][:, tok0 + t0:tok0 + t0 + n], uu[:, :n], uB))
                            while ustores:
                                o_, i_, b_u = ustores.pop(0)
                                S.dma("sp", o_, i_, reads=[b_u])
                            S.emit()

                    longconv(L, 0, 0, 0)
                    longconv(LC, 1, 16, L)

            XH["t"] = tile(gs, "xT2", [128, 8, NTOK], F32)
            for k in range(8):
                S.dma("sp", XH["t"][:, k, :], xs_d[k], writes=xB[k])
            if stop_after >= 3:
                with contextlib.ExitStack() as ph:
                    uT = tile(ph, "uT", [128, 8, NTOK], BF16)
                    uB = Buf()
                    for k in range(8):
                        S.dma("sp", uT[:, k, :], u_d[k], writes=[uB])
                    wst = WS(ph, "outw", [128, 8, 128])
                    tmp = [tile(ph, "otmp", [128, 512], F32) for _ in range(2)]
                    tmpB = [Buf(), Buf()]
                    it = 0
                    for m in range(8):
                        wt, wb = wst.load(out_w_d[:, m * 128:(m + 1) * 128].rearrange("(k p) n -> p k n", p=128))
                        for tb, (t0, n) in enumerate(TBS):
                            s = 0 if tb < 4 else 1
                            pp, ppB = PS[it % 4], PB[it % 4]
                            tt, ttB = tmp[it % 2], tmpB[it % 2]
                            it += 1
                            S.op("pe", MMS([(pp[:, :n], wt[:, k, :], uT[:, k, t0:t0 + n], k == 0, k == 7) for k in range(8)]),
                                 reads=[wb, uB], writes=[ppB])
                            S.op("act", ACT(tt[:, :n], pp[:, :n], AF.Identity, scale=mod[:, 0, 16 + m, s:s + 1], bias=der[:, 0, s, 2, m:m + 1]),
                                 reads=[ppB, modB, derB], writes=[ttB])
                            S.op("dve", TT(XH["t"][:, m, t0:t0 + n], XH["t"][:, m, t0:t0 + n], tt[:, :n], ALU.add), reads=[ttB, xB[m][tb]], writes=[xB[m][tb]])
                    S.emit()
        if stop_after == 3:
            dump()

        def ffn_alloc(ph, ntok=NTOK):
            return dict(w1s=WS(ph, "w1", [128, 8, 128]), w3s=WS(ph, "w3", [128, 8, 128]), w2s=WS(ph, "w2", [128, 7, 128]),
                        ug=tile(ph, "ug", [128, 7, ntok], BF16), ugB=[[Buf() for _ in TBS] for _ in range(7)],
                        sa=[tile(ph, "sa", [128, 512], F32) for _ in range(2)], saB=[Buf(), Buf()], it=[0])

        def ffn(R, hT, hB, tbs, w1d, w3d, w2d, gate_fn):
            w1s, w3s, w2s, ug, ugB, sa, saB = R["w1s"], R["w3s"], R["w2s"], R["ug"], R["ugB"], R["sa"], R["saB"]
            it = R["it"][0]
            for g in range(4):
                for mi in range(7):
                    m = g * 7 + mi
                    a, aB = w1s.load(w1d[:, m * 128:(m + 1) * 128].rearrange("(k p) n -> p k n", p=128))
                    b, bB = w3s.load(w3d[:, m * 128:(m + 1) * 128].rearrange("(k p) n -> p k n", p=128))
                    for tb in tbs:
                        t0, n = TBS[tb]
                        pa, paB = PS[(it % 2) * 2], PB[(it % 2) * 2]
                        pb, pbB = PS[(it % 2) * 2 + 1], PB[(it % 2) * 2 + 1]
                        s_, sB_ = sa[it % 2], saB[it % 2]
                        it += 1
                        S.op("pe", MMS([(pa[:, :n], a[:, k, :], hT[:, k, t0:t0 + n], k == 0, k == 7) for k in range(8)]),
                             reads=[aB, hB[tb]], writes=[paB])
                        S.op("pe", MMS([(pb[:, :n], b[:, k, :], hT[:, k, t0:t0 + n], k == 0, k == 7) for k in range(8)]),
                             reads=[bB, hB[tb]], writes=[pbB])
                        S.op("act", ACT(s_[:, :n], pa[:, :n], AF.Silu), reads=[paB], writes=[sB_])
                        S.op("dve", TT(ug[:, mi, t0:t0 + n], s_[:, :n], pb[:, :n], ALU.mult), reads=[sB_, pbB], writes=[ugB[mi][tb]])
                for m in range(8):
                    w, wB = w2s.load(w2d[g * 896:(g + 1) * 896, m * 128:(m + 1) * 128].rearrange("(k p) n -> p k n", p=128))
                    for tb in tbs:
                        t0, n = TBS[tb]
                        pp, ppB = PS[4 + it % 3], PB[4 + it % 3]
                        it += 1
                        S.op("pe", MMS([(pp[:, :n], w[:, mi, :], ug[:, mi, t0:t0 + n], mi == 0, mi == 6) for mi in range(7)]),
                             reads=[wB] + [ugB[mi][tb] for mi in range(7)], writes=[ppB])
                        gate_fn(m, tb, pp, ppB, t0, n)
            R["it"][0] = it

        if stop_after >= 4:
            with contextlib.ExitStack() as hsc:
                hT = tile(hsc, "hT", [128, 8, NTOK], BF16)
                hB = [Buf() for _ in TBS]
                with contextlib.ExitStack() as ph:
                    norm_mod(ph, range(5), 0, 1, 24, hT, hB, nb=2)
                    S.emit()
                with contextlib.ExitStack() as ph:
                    def gate0(m, tb, pp, ppB, t0, n):
                        s = 0 if tb < 4 else 1
                        S.op("dve", STT(XH["t"][:, m, t0:t0 + n], pp[:, :n], mod[:, 0, 40 + m, s:s + 1], XH["t"][:, m, t0:t0 + n], ALU.mult, ALU.add),
                             reads=[ppB, modB, xB[m][tb]], writes=[xB[m][tb]])
                    ffn(ffn_alloc(ph), hT, hB, range(5), w1_d, w3_d, w2_d, gate0)
                    S.emit()
        if stop_after == 4:
            dump()

        if stop_after >= 5:
            v_d = dscr("v_s", [128, 18, 16, 65], BF16)
            with contextlib.ExitStack() as asc:
                ckn = tile(asc, "ckn", [128, 2, NTOK], BF16)
                cknB = [Buf() for _ in TBS]
                with contextlib.ExitStack() as hsc:
                    hT = tile(hsc, "hT", [128, 8, NTOK], BF16)
                    hB = [Buf() for _ in TBS]
                    with contextlib.ExitStack() as ph:
                        norm_mod(ph, range(5), 1, 0, 0, hT, hB, nb=2)
                        S.emit()
                    with contextlib.ExitStack() as ph:
                        ropeC = tile(ph, "ropeC", [96, L], BF16)
                        ropeS = tile(ph, "ropeS", [96, L], BF16)
                        rpB = Buf()
                        S.dma("sp", ropeC[64:96, :], ropeC_d, writes=[rpB])
                        S.dma("sp", ropeS[64:96, :], ropeS_d, writes=[rpB])
                        wqa = WS(ph, "wqa", [128, 8, 384], nst=1, nbf=1)
                        wa, waB = wqa.load(wqa_d.rearrange("(k p) n -> p k n", p=128))
                        qn = tile(ph, "qn", [128, 3, L], BF16)
                        qnB = [Buf() for _ in range(4)]
                        sq = [tile(ph, "qsq", [128, 3, 512], BF16) for _ in range(2)]
                        sqB = [Buf(), Buf()]
                        rs = [tile(ph, "qrs", [128, 512], F32) for _ in range(2)]
                        rsB = [Buf(), Buf()]
                        t3 = [tile(ph, "qt3", [128, 3, 512], F32) for _ in range(2)]
                        t3B = [Buf(), Buf()]
                        for tb in range(4):
                            t0, n = TBS[tb]
                            for c in range(3):
                                S.op("pe", MMS([(PS[c][:, :], wa[:, k, c * 128:(c + 1) * 128], hT[:, k, t0:t0 + n], k == 0, k == 7) for k in range(8)]),
                                     reads=[waB, hB[tb]], writes=[PB[c]])
                                S.op("act", ACT(sq[tb % 2][:, c, :], PS[c][:, :], AF.Square), reads=[PB[c]], writes=[sqB[tb % 2]])
                            S.op("pe", MMS([(PS[3][:, :], ones_b[:], sq[tb % 2][:, c, :], c == 0, c == 2) for c in range(3)]),
                                 reads=[sqB[tb % 2], cB], writes=[PB[3]])
                            r, rB = rs[tb % 2], rsB[tb % 2]
                            S.op("act", ACT(r[:], PS[3][:, :], AF.Ln, scale=1.0 / 384, bias=V("eps")), reads=[PB[3], vecB], writes=[rB])
                            S.op("act", ACT(r[:], r[:], AF.Exp, scale=-0.5), reads=[rB], writes=[rB])
                            for c in range(3):
                                S.op("dve", TT(t3[tb % 2][:, c, :], PS[c][:, :], r[:], ALU.mult), reads=[PB[c], rB], writes=[t3B[tb % 2]])
                                S.op("act", ACT(qn[:, c, t0:t0 + n], t3[tb % 2][:, c, :], AF.Identity, scale=V("qnorm", c)),
                                     reads=[t3B[tb % 2], vecB], writes=[qnB[tb]])
                        wqb = WS(ph, "wqb", [128, 3, 128])
                        ropeCS = tile(ph, "ropeCS", [128, L], BF16)
                        shm = tile(ph, "shm", [128, 96], BF16)
                        S.dma("sp", ropeCS[:], ropeCS_d, writes=[rpB])
                        S.dma("sp", shm[:], shift_d, writes=[rpB])
                        tT = [tile(ph, "tT", [128, 512], BF16) for _ in range(3)]
                        tTB = [Buf() for _ in range(3)]
                        qo = [tile(ph, "qo", [96, 512], BF16) for _ in range(3)]
                        qoB = [Buf() for _ in range(3)]
                        it = 0
                        qpref = [wqb.issue(wqb2_d[:, 0, :].rearrange("(k p) n -> p k n", p=128))]
                        for h in range(16):
                            w, wB = wqb.finish(qpref.pop(0))
                            if h + 1 < 16:
                                qpref.append(wqb.issue(wqb2_d[:, h + 1, :].rearrange("(k p) n -> p k n", p=128)))
                            for tb in range(4):
                                t0, n = TBS[tb]
                                i2 = it % 3
                                it += 1
                                pq_, pqB_ = PS[i2], PB[i2]
                                p2_, p2B_ = PS[3 + i2], PB[3 + i2]
                                S.op("pe", MMS([(pq_[:, :], w[:, c, :], qn[:, c, t0:t0 + n], c == 0, c == 2) for c in range(3)]),
                                     reads=[wB, qnB[tb]], writes=[pqB_])
                                S.op("dve", TT(tT[i2][:], pq_[:, :], ropeCS[:, t0:t0 + n], ALU.mult), reads=[pqB_, rpB], writes=[tTB[i2]])
                                S.op("pe", MMS([(p2_[0:96, :], shm[:], tT[i2][:], True, True)]), reads=[rpB, tTB[i2]], writes=[p2B_])
                                S.op("act", ACT(qo[i2][:], p2_[0:96, :], AF.Copy), reads=[p2B_], writes=[qoB[i2]])
                                S.dma("sp", q_d[h][:, t0:t0 + n], qo[i2][:], reads=[qoB[i2]])
                        S.emit()
                    with contextlib.ExitStack() as ph:
                        ropeC = tile(ph, "ropeC", [96, L], BF16)
                        ropeS = tile(ph, "ropeS", [96, L], BF16)
                        rpB = Buf()
                        S.dma("sp", ropeC[64:96, :], ropeC_d, writes=[rpB])
                        S.dma("sp", ropeS[64:96, :], ropeS_d, writes=[rpB])
                        wkva = WS(ph, "wkva", [128, 8, 256], nst=1, nbf=1)
                        wa, waB = wkva.load(wkva_d.rearrange("(k p) n -> p k n", p=128))
                        wkp = WS(ph, "wkp", [128, 8, 96], nst=2, nbf=2)
                        wp, wpB = wkp.load(wkpe_d.rearrange("(k p) n -> p k n", p=128))
                        wps, wpsB = wkp.load(wkpes_d.rearrange("(k p) n -> p k n", p=128))
                        kpe = tile(ph, "kpe", [96, NTOK], BF16)
                        kpeB = Buf()
                        sq = [tile(ph, "ksq", [128, 2, 512], BF16) for _ in range(2)]
                        sqB = [Buf(), Buf()]
                        rs = [tile(ph, "krs", [128, 512], F32) for _ in range(2)]
                        rsB = [Buf(), Buf()]
                        t3 = [tile(ph, "kt3", [128, 2, 512], F32) for _ in range(2)]
                        t3B = [Buf(), Buf()]
                        ka_t = [tile(ph, "ka_t", [96, 512], F32) for _ in range(2)]
                        kb_t = [tile(ph, "kb_t", [96, 512], F32) for _ in range(2)]
                        kaB, kbB = [Buf(), Buf()], [Buf(), Buf()]
                        for tb, (t0, n) in enumerate(TBS):
                            i2 = tb % 2
                            for c in range(2):
                                S.op("pe", MMS([(PS[c][:, :n], wa[:, k, c * 128:(c + 1) * 128], hT[:, k, t0:t0 + n], k == 0, k == 7) for k in range(8)]),
                                     reads=[waB, hB[tb]], writes=[PB[c]])
                                S.op("act", ACT(sq[i2][:, c, :n], PS[c][:, :n], AF.Square), reads=[PB[c]], writes=[sqB[i2]])
                            S.op("pe", MMS([(PS[2][:, :n], ones_b[:], sq[i2][:, c, :n], c == 0, c == 1) for c in range(2)]),
                                 reads=[sqB[i2], cB], writes=[PB[2]])
                            r, rB = rs[i2], rsB[i2]
                            S.op("act", ACT(r[:, :n], PS[2][:, :n], AF.Ln, scale=1.0 / 256, bias=V("eps")), reads=[PB[2], vecB], writes=[rB])
                            S.op("act", ACT(r[:, :n], r[:, :n], AF.Exp, scale=-0.5), reads=[rB], writes=[rB])
                            for c in range(2):
                                S.op("dve", TT(t3[i2][:, c, :n], PS[c][:, :n], r[:, :n], ALU.mult), reads=[PB[c], rB], writes=[t3B[i2]])
                                S.op("act", ACT(ckn[:, c, t0:t0 + n], t3[i2][:, c, :n], AF.Identity, scale=V("kvnorm", c)),
                                     reads=[t3B[i2], vecB], writes=[cknB[tb]])
                            S.op("pe", MMS([(PS[3][0:96, :n], wp[:, k, :], hT[:, k, t0:t0 + n], k == 0, k == 7) for k in range(8)]),
                                 reads=[wpB, hB[tb]], writes=[PB[3]])
                            if tb < 4:
                                S.op("pe", MMS([(PS[4][0:96, :n], wps[:, k, :], hT[:, k, t0:t0 + n], k == 0, k == 7) for k in range(8)]),
                                     reads=[wpsB, hB[tb]], writes=[PB[4]])
                                S.op("dve", TT(ka_t[i2][64:96, :n], PS[3][64:96, :n], ropeC[64:96, t0:t0 + n], ALU.mult), reads=[PB[3], rpB], writes=[kaB[i2]])
                                S.op("dve", TT(kb_t[i2][64:96, :n], PS[4][64:96, :n], ropeS[64:96, t0:t0 + n], ALU.mult), reads=[PB[4], rpB], writes=[kbB[i2]])
                                S.op("dve", TT(kpe[64:96, t0:t0 + n], ka_t[i2][64:96, :n], kb_t[i2][64:96, :n], ALU.add), reads=[kaB[i2], kbB[i2]], writes=[kpeB])
                            else:
                                S.op("act", ACT(kpe[64:96, t0:t0 + n], PS[3][64:96, :n], AF.Copy), reads=[PB[3]], writes=[kpeB])
                        for h in range(16):
                            S.dma("sp", k_d[h][64:96, :], kpe[64:96, :], reads=[kpeB])
                        wkb = WS(ph, "wkb", [128, 2, 128])
                        ko = [tile(ph, "ko", [128, NTOK], BF16) for _ in range(2)]
                        koB = [Buf(), Buf()]
                        it = 0
                        kpref = [wkb.issue(wkbn_d[:, 0:2, :].rearrange("(k p) h n -> p k (h n)", p=128))]
                        for hp in range(8):
                            w, wB = wkb.finish(kpref.pop(0))
                            if hp + 1 < 8:
                                kpref.append(wkb.issue(wkbn_d[:, 2 * hp + 2:2 * hp + 4, :].rearrange("(k p) h n -> p k (h n)", p=128)))
                            for tb, (t0, n) in enumerate(TBS):
                                pp, ppB = PS[it % 4], PB[it % 4]
                                it += 1
                                S.op("pe", MMS([(pp[:, :n], w[:, c, :], ckn[:, c, t0:t0 + n], c == 0, c == 1) for c in range(2)]),
                                     reads=[wB, cknB[tb]], writes=[ppB])
                                S.op("act" if tb % 2 == 0 else "dve",
                                     ACT(ko[hp % 2][:, t0:t0 + n], pp[:, :n], AF.Copy) if tb % 2 == 0 else CP(ko[hp % 2][:, t0:t0 + n], pp[:, :n]),
                                     reads=[ppB], writes=[koB[hp % 2]])
                            S.dma("sp", k_d[2 * hp][0:64, :], ko[hp % 2][0:64, :], reads=[koB[hp % 2]])
                            S.dma("sp", k_d[2 * hp + 1][0:64, :], ko[hp % 2][64:128, :], reads=[koB[hp % 2]])
                        S.emit()
                with contextlib.ExitStack() as ph:
                        V_all = tile(ph, "Vall", [128, 18, 16, 65], BF16)
                        VB = [Buf() for _ in range(18)]
                        wv = WS(ph, "wvb", [128, 2, 1024], nst=1, nbf=1)
                        wvt, wvB = wv.load(wvb_d.rearrange("(k p) n -> p k n", p=128))
                        for kc in range(18):
                            tb = min(kc // 4, 4)
                            S.op("pool", MSET(V_all[:, kc, :, 64:65], 1.0), writes=[VB[kc]])
                            for hf in range(2):
                                pp, ppB = PS[(kc * 2 + hf) % 4], PB[(kc * 2 + hf) % 4]
                                S.op("pe", MMS([(pp[:, :], ckn[:, c, kc * 128:(kc + 1) * 128], wvt[:, c, hf * 512:(hf + 1) * 512], c == 0, c == 1) for c in range(2)]),
                                     reads=[wvB, cknB[tb]], writes=[ppB])
                                S.op("act" if hf == 0 else "dve",
                                     (ACT if hf == 0 else (lambda o, i, f: CP(o, i)))(V_all[:, kc, hf * 8:(hf + 1) * 8, 0:64], pp[:, :].rearrange("p (h d) -> p h d", h=8), AF.Copy),
                                     reads=[ppB], writes=[VB[kc]])
                        S.dma("sp", v_d, V_all[:], reads=VB)
                        S.emit()
            if True:
                with contextlib.ExitStack() as ph:
                    V_all = tile(ph, "Vall2", [128, 18, 16, 65], BF16)
                    VB = [Buf() for _ in range(18)]
                    S.dma("sp", V_all[:], v_d, writes=VB)
                    KT = [tile(ph, "KT", [96, NTOK], BF16) for _ in range(2)]
                    QT = [tile(ph, "QT", [96, L], BF16) for _ in range(2)]
                    KTB, QTB = [Buf(), Buf()], [Buf(), Buf()]
                    PT = [tile(ph, "PT", [128, 512], BF16) for _ in range(3)]
                    PTB = [Buf() for _ in range(3)]
                    rd = [tile(ph, "rd", [65, 512], F32) for _ in range(2)]
                    rdB = [Buf(), Buf()]
                    bc = [tile(ph, "bc", [64, 512], F32) for _ in range(2)]
                    bcB = [Buf(), Buf()]
                    ao = [tile(ph, "ao", [64, L], BF16) for _ in range(2)]
                    aoB = [Buf(), Buf()]
                    scale = 1.0 / float(np.sqrt(96.0))
                    iters = [(h, qb, kc) for h in range(16) for qb in range(4) for kc in range(18)]
                    nit = len(iters)
                    pending = []

                    def load_head(h):
                        S.dma("sp", KT[h % 2][:], k_d[h], writes=[KTB[h % 2]])
                        S.dma("sp", QT[h % 2][:], q_d[h], writes=[QTB[h % 2]])

                    def finalize(g):
                        h, qb = g // 4, g % 4
                        po, poB = PS[4 + g % 2], PB[4 + g % 2]
                        r_, rB_ = rd[g % 2], rdB[g % 2]
                        b_, bB_ = bc[g % 2], bcB[g % 2]
                        S.op("pe", MMS([(PS[6][0:64, :], ones_f[64:65, 0:64], r_[64:65, :], True, True)]), reads=[rB_, cB], writes=[PB[6]])
                        S.op("dve", CP(b_[:], PS[6][0:64, :]), reads=[PB[6]], writes=[bB_])
                        S.op("dve", TT(ao[h % 2][:, qb * 512:(qb + 1) * 512], po[0:64, :], b_[:], ALU.mult), reads=[poB, bB_], writes=[aoB[h % 2]])
                        if qb == 3:
                            S.dma("sp", ao_d[h], ao[h % 2][:], reads=[aoB[h % 2]])

                    load_head(0)
                    LOOK = 2
                    for j in range(nit + LOOK):
                        if j < nit:
                            h, qb, kc = iters[j]
                            if qb == 0 and kc == 0 and h + 1 < 16:
                                load_head(h + 1)
                            kt, ktB, qt, qtB = KT[h % 2], KTB[h % 2], QT[h % 2], QTB[h % 2]
                            pS, pSB, pt, ptB = PS[j % 3], PB[j % 3], PT[j % 3], PTB[j % 3]
                            S.op("pe", MMS([(pS[:, :], kt[:, kc * 128:(kc + 1) * 128], qt[:, qb * 512:(qb + 1) * 512], True, True)]),
                                 reads=[ktB, qtB], writes=[pSB])
                            S.op("act", ACT(pt[:], pS[:, :], AF.Exp, scale=scale), reads=[pSB], writes=[ptB])
                        jj = j - LOOK
                        if jj >= 0:
                            h, qb, kc = iters[jj]
                            g = h * 4 + qb
                            po, poB = PS[4 + g % 2], PB[4 + g % 2]
                            pt, ptB = PT[jj % 3], PTB[jj % 3]
                            S.op("pe", MMS([(po[0:65, :], V_all[:, kc, h, :], pt[:], kc == 0, kc == 17)]), reads=[VB[kc], ptB], writes=[poB])
                            if kc == 17:
                                r_, rB_ = rd[g % 2], rdB[g % 2]
                                S.op("dve", lambda e, r_=r_, po=po: e.reciprocal(out=r_[64:65, :], in_=po[64:65, :]), reads=[poB], writes=[rB_])
                                pending.append((j + 9, g))
                        while pending and pending[0][0] <= j:
                            finalize(pending.pop(0)[1])
                    while pending:
                        finalize(pending.pop(0)[1])
                    S.emit()
            with contextlib.ExitStack() as ph:
                wos = WS(ph, "wo", [128, 8, 128])
                aob = tile(ph, "aob", [128, 8, L], BF16)
                aobB = [Buf() for _ in range(4)]
                aov = ao_d.rearrange("(c two) d t -> two d c t", two=2)
                for tb in range(4):
                    t0, n = TBS[tb]
                    for two in range(2):
                        S.dma("sp", aob[two * 64:(two + 1) * 64, :, t0:t0 + n], aov[two][:, :, t0:t0 + n], writes=[aobB[tb]])
                it = 0
                wov = wo_d.rearrange("h d n -> (h d) n")
                for m in range(8):
                    wt, wB = wos.load(wov[:, m * 128:(m + 1) * 128].rearrange("(c p) n -> p c n", p=128))
                    for tb in range(4):
                        t0, n = TBS[tb]
                        pp, ppB = PS[it % 4], PB[it % 4]
                        it += 1
                        S.op("pe", MMS([(pp[:, :], wt[:, c, :], aob[:, c, t0:t0 + n], c == 0, c == 7) for c in range(8)]),
                             reads=[wB, aobB[tb]], writes=[ppB])
                        S.op("dve", STT(XH["t"][:, m, t0:t0 + n], pp[:, :], mod[:, 1, 16 + m, 0:1], XH["t"][:, m, t0:t0 + n], ALU.mult, ALU.add),
                             reads=[ppB, modB, xB[m][tb]], writes=[xB[m][tb]])
                S.emit()
        if stop_after == 5:
            dump()

        if stop_after >= 6:
            with contextlib.ExitStack() as hsc:
                hT = tile(hsc, "hT", [128, 8, L], BF16)
                hB = [Buf() for _ in range(4)]
                gT = tile(hsc, "gT", [8, L], F32)
                gTB = Buf()
                selt = tile(hsc, "selt", [8, 8, 128], F32)
                selB = Buf()
                S.dma("sp", selt[:], sel_d, writes=[selB])
                with contextlib.ExitStack() as ph:
                    rt = tile(ph, "rt", [128, 8, NE], F32)
                    rtB = Buf()
                    S.dma("sp", rt[:], rout_d.rearrange("(k p) e -> p k e", p=128), writes=[rtB])
                    h32 = [tile(ph, "h32", [128, 8, 512], F32) for _ in range(2)]
                    h32B = [Buf(), Buf()]
                    lg = tile(ph, "lg", [128, 16, 8], F32)
                    mx = tile(ph, "mx", [128, 16, 8], F32)
                    gs_ = tile(ph, "gs", [128, 16, 8], F32)
                    sm = tile(ph, "sm", [128, 16, 4], F32)
                    gB = [Buf() for _ in range(16)]

                    def router(i, tb, t, tB):
                        for c4 in range(4):
                            tc = tb * 4 + c4
                            pp, ppB = PS[2 + tc % 2], PB[2 + tc % 2]
                            S.op("pe", MMS([(pp[:, 0:8], t[:, k, c4 * 128:(c4 + 1) * 128], rt[:, k, :], k == 0, k == 7) for k in range(8)]),
                                 reads=[tB, rtB], writes=[ppB])
                            S.op("dve", CP(lg[:, tc, :], pp[:, 0:8]), reads=[ppB], writes=[gB[tc]])
                            S.op("dve", lambda e, tc=tc: e.max(out=mx[:, tc, :], in_=lg[:, tc, :]), reads=[gB[tc]], writes=[gB[tc]])
                            S.op("dve", TS(sm[:, tc, 0:1], mx[:, tc, 0:1], -1.0, None, ALU.mult), reads=[gB[tc]], writes=[gB[tc]])
                            S.op("act", ACT(gs_[:, tc, :], lg[:, tc, :], AF.Exp, bias=sm[:, tc, 0:1]), reads=[gB[tc]], writes=[gB[tc]])
                            S.op("dve", STT(gs_[:, tc, :], lg[:, tc, :], mx[:, tc, 1:2], gs_[:, tc, :], ALU.is_ge, ALU.mult), reads=[gB[tc]], writes=[gB[tc]])
                            S.op("dve", lambda e, tc=tc: e.reduce_sum(out=sm[:, tc, 1:2], in_=gs_[:, tc, :], axis=mybir.AxisListType.X), reads=[gB[tc]], writes=[gB[tc]])
                            S.op("dve", lambda e, tc=tc: e.reciprocal(out=sm[:, tc, 2:3], in_=sm[:, tc, 1:2]), reads=[gB[tc]], writes=[gB[tc]])
                            S.op("dve", TS(gs_[:, tc, :], gs_[:, tc, :], sm[:, tc, 2:3], None, ALU.mult), reads=[gB[tc]], writes=[gB[tc]])
                            S.op("pe", TR(PS[4 + tc % 2][0:8, 0:128], gs_[:, tc, :], ident_f[:]), reads=[gB[tc], cB], writes=[PB[4 + tc % 2]])
                            S.op("act", ACT(gT[:, tc * 128:(tc + 1) * 128], PS[4 + tc % 2][0:8, 0:128], AF.Copy), reads=[PB[4 + tc % 2]], writes=[gTB])

                    norm_mod(ph, range(4), 1, 1, 24, hT, hB, h32=h32, h32B=h32B, after=router, nb=2)
                    S.emit()
                with contextlib.ExitStack() as ph:
                    gbc = [tile(ph, "gbc", [128, L], BF16) for _ in range(2)]
                    gbcB = [Buf(), Buf()]
                    tmp = [tile(ph, "mtmp", [128, 512], F32) for _ in range(2)]
                    tmpB = [Buf(), Buf()]
                    ti = [0]
                    FR = ffn_alloc(ph, L)
                    for e_ in range(NE):
                        g_, gB_ = gbc[e_ % 2], gbcB[e_ % 2]
                        for tb in range(4):
                            t0, n = TBS[tb]
                            S.op("pe", MMS([(PS[6][:, :], selt[:, e_, :], gT[:, t0:t0 + n], True, True)]), reads=[selB, gTB], writes=[PB[6]])
                            S.op("act", CPACT(g_[:, t0:t0 + n], PS[6][:, :]), reads=[PB[6]], writes=[gB_])

                        def gate1(m, tb, pp, ppB, t0, n, g_=g_, gB_=gB_):
                            tt, ttB = tmp[ti[0] % 2], tmpB[ti[0] % 2]
                            ti[0] += 1
                            S.op("dve", STT(tt[:, :n], pp[:, :n], mod[:, 1, 40 + m, 0:1], g_[:, t0:t0 + n], ALU.mult, ALU.mult),
                                 reads=[ppB, modB, gB_], writes=[ttB])
                            S.op("dve", TT(XH["t"][:, m, t0:t0 + n], XH["t"][:, m, t0:t0 + n], tt[:, :n], ALU.add), reads=[ttB, xB[m][tb]], writes=[xB[m][tb]])
                        ffn(FR, hT, hB, range(4), mw1_d[e_], mw3_d[e_], mw2_d[e_], gate1)
                    S.emit()
        if stop_after == 6:
            dump()

        if stop_after >= 7:
            with contextlib.ExitStack() as ph:
                sq = [tile(ph, "fsq", [128, 8, 512], BF16) for _ in range(2)]
                sqB = [Buf(), Buf()]
                rs = [tile(ph, "frs", [128, 512], F32) for _ in range(2)]
                rsB = [Buf(), Buf()]
                ot = [tile(ph, "fot", [128, 8, 512], F32) for _ in range(2)]
                otB = [Buf(), Buf()]
                for tb in range(4):
                    t0, n = TBS[tb]
                    i2 = tb % 2
                    S.op("act", ACT(sq[i2][:], XH["t"][:, :, t0:t0 + n], AF.Square), reads=[xB[k][tb] for k in range(8)], writes=[sqB[i2]])
                    S.op("pe", MMS([(PS[i2][:, :], ones_b[:], sq[i2][:, k, :], k == 0, k == 7) for k in range(8)]), reads=[sqB[i2], cB], writes=[PB[i2]])
                    S.op("act", ACT(rs[i2][:], PS[i2][:, :], AF.Ln, scale=1.0 / D, bias=V("eps")), reads=[PB[i2], vecB], writes=[rsB[i2]])
                    S.op("act", ACT(rs[i2][:], rs[i2][:], AF.Exp, scale=-0.5), reads=[rsB[i2]], writes=[rsB[i2]])
                    for k in range(8):
                        S.op("dve", STT(ot[i2][:, k, :], XH["t"][:, k, t0:t0 + n], V("nfin", k), rs[i2][:], ALU.mult, ALU.mult),
                             reads=[xB[k][tb], vecB, rsB[i2]], writes=[otB[i2]])
                    for k in range(8):
                        S.dma("sp", yT_d[k * 128:(k + 1) * 128, t0:t0 + n], ot[i2][:, k, :], reads=[otB[i2]])
                S.emit()
    return nc


def CPACT(out, in_):
    return lambda e: e.activation(out=out, in_=in_, func=AF.Copy)


def _bf(a):
    return np.ascontiguousarray(a.astype(ml_dtypes.bfloat16))


def _dft_tables(Lf):
    NT = Lf // 128
    N2 = 2 * Lf
    s = np.arange(Lf, dtype=np.float64)
    f = np.arange(Lf, dtype=np.float64) + 0.5
    ang = 2 * np.pi * np.outer(s, f) / N2
    C, Sn = np.cos(ang), np.sin(ang)
    def fwd(M):
        return M.reshape(NT, 128, NT, 128).transpose(2, 1, 0, 3)
    F = np.stack([fwd(C), fwd(Sn)], axis=2)
    def inv(M):
        return M.T.reshape(NT, 128, Lf)
    I = np.stack([inv(C), inv(Sn)], axis=2)
    return _bf(F), _bf(I)


def _zpos(Lf):
    pos = np.arange(Lf, dtype=np.float32)
    t = (pos / max(Lf - 1, 1))[:, None]
    w = (2.0 * np.pi * pos / Lf).astype(np.float32)
    f = np.linspace(1e-4, 7, 8, dtype=np.float32)
    ang = w[:, None] * f[None, :]
    z = np.concatenate([t, np.cos(ang), -np.sin(ang)], axis=-1).astype(np.float32)
    return np.ascontiguousarray(z.T)


def _cols(v):
    v = np.asarray(v, np.float32).reshape(-1)
    n = v.size
    if n < 128:
        o = np.zeros((128, 1), np.float32)
        o[:n, 0] = v
        return o
    return np.ascontiguousarray(v.reshape(n // 128, 128).T)


_CONST = {}


def _constants():
    if _CONST:
        return _CONST
    Fx, Ix = _dft_tables(L)
    Fc, Ic = _dft_tables(LC)
    deltas = np.abs(np.linspace(np.log(1e-2) / 1.5, np.log(1e-2) / 0.3, D, dtype=np.float32))
    rows = L // 64
    row = np.broadcast_to(np.arange(rows, dtype=np.float32)[:, None], (rows, 64)).reshape(L)
    col = np.broadcast_to(np.arange(64, dtype=np.float32)[None, :], (rows, 64)).reshape(L)
    inv = (10000.0 ** (-np.arange(0, 16, 2, dtype=np.float32) / 16)).astype(np.float32)
    ang = np.concatenate([row[:, None] * inv, col[:, None] * inv], axis=-1)
    cosT, sinT = np.cos(ang).T, np.sin(ang).T
    rc = np.repeat(cosT, 2, axis=0)
    rsn = np.repeat(sinT, 2, axis=0)
    rsn[0::2] *= -1.0
    sel = np.zeros((8, 8, 128), np.float32)
    for e in range(8):
        sel[e, e, :] = 1.0
    _CONST.update(dict(
        dftf_x=Fx, dfti_x=Ix, dftf_c=Fc, dfti_c=Ic,
        zpos_x=_zpos(L), zpos_c=_zpos(LC),
        delta_bc=np.ascontiguousarray(np.broadcast_to(deltas[None, :], (128, D))).astype(np.float32),
        ropeC=_bf(rc), ropeS=_bf(rsn), sel=sel,
        ropeCS=_bf(np.concatenate([np.ones((64, L), np.float32), rc, rsn], axis=0)),
        shiftm=_bf(np.concatenate([np.eye(96, dtype=np.float32), np.eye(96, dtype=np.float32)[64:96]], axis=0)),
        tnx=_cols(-np.arange(L, dtype=np.float32) / (L - 1)), tnc=_cols(-np.arange(LC, dtype=np.float32) / (LC - 1)),
    ))
    return _CONST


def _swap_pairs(a):
    sh = a.shape
    return np.ascontiguousarray(a.reshape(sh[:-1] + (sh[-1] // 2, 2))[..., ::-1].reshape(sh))


def make_in_maps(inp, ncores=8, need_moe=True):
    C = _constants()
    f32 = lambda a: np.ascontiguousarray(np.asarray(a, np.float32))
    wq_b = f32(inp["mla_wq_b"][0]).reshape(384, 16, 96)
    wq_b_sw = wq_b.copy()
    wq_b_sw[:, :, 64:] = _swap_pairs(wq_b[:, :, 64:])
    wkv_a = f32(inp["mla_wkv_a"][0])
    wkpe = np.concatenate([wkv_a[:, 0:64], wkv_a[:, 256:288]], axis=1)
    wkpe_sw = np.concatenate([wkv_a[:, 0:64], _swap_pairs(wkv_a[:, 256:288])], axis=1)
    wkv_b = f32(inp["mla_wkv_b"][0]).reshape(256, 16, 128)
    shared = dict(
        fbias_row=f32(inp["hy_f_bias"][0]).reshape(1, D), delta_bc=C["delta_bc"],
        zpos_x=C["zpos_x"], zpos_c=C["zpos_c"], dftf_x=C["dftf_x"], dftf_c=C["dftf_c"],
        dfti_x=C["dfti_x"], dfti_c=C["dfti_c"], ropeC=C["ropeC"], ropeS=C["ropeS"], sel=C["sel"],
        ropeCS=C["ropeCS"], shiftm=C["shiftm"], wq_b2=np.ascontiguousarray(np.concatenate([wq_b, wq_b_sw[:, :, 64:]], axis=2)),
        ada_w=f32(inp["ada_w"]), hy_in_w=f32(inp["hy_in_w"][0]), hy_f_w0=f32(inp["hy_f_w0"][0]),
        hy_f_wi=f32(inp["hy_f_wi"][0]), hy_f_wout=f32(inp["hy_f_wout"][0]), hy_out_w=f32(inp["hy_out_w"][0]),
        ffn_w1=f32(inp["ffn_w1"][0]), ffn_w3=f32(inp["ffn_w3"][0]), ffn_w2=f32(inp["ffn_w2"][0]),
        wq_a=f32(inp["mla_wq_a"][0]),
        wkv_a=np.ascontiguousarray(wkv_a[:, 0:256]), wkpe=np.ascontiguousarray(wkpe), wkpe_sw=np.ascontiguousarray(wkpe_sw),
        wkb_nope=np.ascontiguousarray(wkv_b[:, :, 0:64]), wvb=np.ascontiguousarray(wkv_b[:, :, 64:128].reshape(256, 1024)),
        wo=f32(inp["mla_wo"][0]).reshape(16, 64, D), router=f32(inp["moe_router"][0]),
        moe_w1=f32(inp["moe_w1"][0]), moe_w3=f32(inp["moe_w3"][0]), moe_w2=f32(inp["moe_w2"][0]),
    )
    maps = []
    for b in range(ncores):
        vec = np.zeros((128, NV), np.float32)

        def put(name, arr):
            o, c = VOFF[name]
            assert arr.shape == (128, c), (name, arr.shape, c)
            vec[:, o:o + c] = arr
        cc = _cols(inp["c"][b])
        cx = _cols(inp["c_ctx"])
        c2 = np.zeros((128, 16), np.float32)
        c2[:, 0::2] = cc
        c2[:, 1::2] = cx
        put("c2", c2)
        put("adab0", _cols(inp["ada_b"][0]))
        put("adab1", _cols(inp["ada_b"][1]))
        for l in range(2):
            put(f"nmix{l}", _cols(inp["norm_mix"][l]))
            put(f"nffn{l}", _cols(inp["norm_ffn"][l]))
        put("inb", _cols(inp["hy_in_b"][0]))
        for j in range(3):
            put(f"scw{j}", _cols(inp["hy_sc_w"][0][j]))
        put("scb", _cols(inp["hy_sc_b"][0]))
        put("outb", _cols(inp["hy_out_b"][0]))
        put("qnorm", _cols(inp["mla_q_norm"][0]))
        put("kvnorm", _cols(inp["mla_kv_norm"][0]))
        put("nfin", _cols(inp["norm_final"]))
        put("fb0", _cols(inp["hy_f_b0"][0]))
        put("fbi0", _cols(inp["hy_f_bi"][0][0]))
        put("fbi1", _cols(inp["hy_f_bi"][0][1]))
        put("ffreq", _cols(inp["hy_f_freq"][0]))
        put("eps", np.full((128, 1), 1e-6, np.float32))
        put("tnx", C["tnx"])
        put("tnc", C["tnc"])
        m = dict(shared)
        m["xT"] = np.ascontiguousarray(np.asarray(inp["x"][b], np.float32).T)
        m["cT"] = np.ascontiguousarray(np.asarray(inp["ctx"][b], np.float32).T)
        m["vec"] = vec
        maps.append(m)
    return maps


_NC = {}


def kernel(**inputs):
    if "nc" not in _NC:
        _NC["nc"] = build()
    maps = make_in_maps(inputs)
    res = run_bass_kernel_spmd(_NC["nc"], maps, core_ids=list(range(8)))
    out = np.stack([np.ascontiguousarray(res.results[b]["yT"].T) for b in range(8)], axis=0)
    return out.astype(np.float32)
```
